# Optimizing a Trainium2 kernel written in Bass

```python
import functools
import jax, jax.numpy as jnp
from jax import lax
import numpy as np

D_MODEL = 1024
BATCH = 4
SEQ = 4096
DEPTH = 2

HEAD_DIM = 64
N_MOBA_HEADS = 8
N_FOX_HEADS = 8
N_ATTN_HEADS = N_MOBA_HEADS + N_FOX_HEADS
ATTN_WIDTH = N_ATTN_HEADS * HEAD_DIM
ROPE_DIMS = HEAD_DIM // 4
ROPE_THETA = 500000.0
MOBA_BLOCK = 256
MOBA_TOPK = 3
MOBA_QCHUNK = 64
FOX_QBLOCK = 128
FOX_BIAS_MEAN = 2.0
D_RNN = 1280
N_RNN_BLOCKS = 10
RNN_BLOCK_W = D_RNN // N_RNN_BLOCKS
CONV_WIDTH = 4
RG_C = 8.0
D_FF = 2816
N_EXPERTS = 8
TOP_K = 2
D_EXPERT = 3584
MOE_BLOCK = 256
N_EVEN_LAYERS = (DEPTH + 1) // 2
N_ODD_LAYERS = DEPTH // 2
NORM_EPS = 1e-6

kernel_name = "hybrid_moba_fox_rglru_moe"


def rms_norm(x, g):
    xf = x.astype(jnp.float32)
    y = xf * lax.rsqrt(jnp.mean(xf * xf, axis=-1, keepdims=True) + NORM_EPS)
    return (y * g.astype(jnp.float32)).astype(x.dtype)


def apply_partial_rope(x, pos):
    half = ROPE_DIMS // 2
    inv_freq = jnp.power(ROPE_THETA, -jnp.arange(half, dtype=jnp.float32) / half)
    ang = pos.astype(jnp.float32)[:, None] * inv_freq[None, :]
    cos, sin = jnp.cos(ang), jnp.sin(ang)
    x1 = x[..., :half].astype(jnp.float32)
    x2 = x[..., half:ROPE_DIMS].astype(jnp.float32)
    rot = jnp.concatenate([x1 * cos - x2 * sin, x2 * cos + x1 * sin], axis=-1).astype(x.dtype)
    return jnp.concatenate([rot, x[..., ROPE_DIMS:]], axis=-1)


def moba_attention(q, k, v):
    b, h, s, dh = q.shape
    n_blk = -(-s // MOBA_BLOCK)
    s_pad = n_blk * MOBA_BLOCK
    pad = ((0, 0), (0, 0), (0, s_pad - s), (0, 0))
    q, k, v = (jnp.pad(t, pad) for t in (q, k, v))
    k_blocks = k.reshape(b, h, n_blk, MOBA_BLOCK, dh)
    v_blocks = v.reshape(b, h, n_blk, MOBA_BLOCK, dh)
    k_mean = jnp.mean(k_blocks.astype(jnp.float32), axis=3)
    q_block_id = jnp.arange(s_pad) // MOBA_BLOCK
    gate = jnp.einsum('bhsd,bhnd->bhsn', q.astype(jnp.float32), k_mean)
    fully_past = jnp.arange(n_blk)[None, :] < q_block_id[:, None]
    gate = jnp.where(fully_past, gate, -jnp.inf)
    k_sel = min(MOBA_TOPK, n_blk)
    _, sel = lax.top_k(gate, k_sel)
    n_chunks = s_pad // MOBA_QCHUNK
    q_chunks = q.reshape(b, h, n_chunks, MOBA_QCHUNK, dh).transpose(2, 0, 1, 3, 4)
    sel_chunks = sel.reshape(b, h, n_chunks, MOBA_QCHUNK, k_sel).transpose(2, 0, 1, 3, 4)
    scale = HEAD_DIM ** -0.5
    gather = jax.vmap(jax.vmap(lambda blocks, idx: blocks[idx]))

    def one_chunk(args):
        ci, q_c, sel_c = args
        q_start = ci * MOBA_QCHUNK
        blk = q_start // MOBA_BLOCK
        k_own = lax.dynamic_index_in_dim(k_blocks, blk, axis=2, keepdims=False)
        v_own = lax.dynamic_index_in_dim(v_blocks, blk, axis=2, keepdims=False)
        q_pos = q_start + jnp.arange(MOBA_QCHUNK)
        k_pos = blk * MOBA_BLOCK + jnp.arange(MOBA_BLOCK)
        s_own = jnp.einsum('bhqd,bhkd->bhqk', q_c, k_own, preferred_element_type=jnp.float32) * scale
        s_own = jnp.where(k_pos[None, :] <= q_pos[:, None], s_own, -jnp.inf)
        k_g = gather(k_blocks, sel_c)
        v_g = gather(v_blocks, sel_c)
        s_sel = jnp.einsum('bhqd,bhqnkd->bhqnk', q_c, k_g, preferred_element_type=jnp.float32) * scale
        s_sel = jnp.where((sel_c < blk)[..., None], s_sel, -jnp.inf)
        scores = jnp.concatenate([s_own, s_sel.reshape(b, h, MOBA_QCHUNK, k_sel * MOBA_BLOCK)], axis=-1)
        p = jax.nn.softmax(scores, axis=-1).astype(v.dtype)
        p_own = p[..., :MOBA_BLOCK]
        p_sel = p[..., MOBA_BLOCK:].reshape(b, h, MOBA_QCHUNK, k_sel, MOBA_BLOCK)
        return (jnp.einsum('bhqk,bhkd->bhqd', p_own, v_own)
                + jnp.einsum('bhqnk,bhqnkd->bhqd', p_sel, v_g))

    out = lax.map(one_chunk, (jnp.arange(n_chunks), q_chunks, sel_chunks))
    out = out.transpose(1, 2, 0, 3, 4).reshape(b, h, s_pad, dh)
    return out[:, :, :s]


def forgetting_attention(q, k, v, log_f):
    b, h, s, dh = q.shape
    cum = jnp.cumsum(log_f, axis=-1)
    n_qb = s // FOX_QBLOCK
    q_blocks = q.reshape(b, h, n_qb, FOX_QBLOCK, dh).transpose(2, 0, 1, 3, 4)
    cum_blocks = cum.reshape(b, h, n_qb, FOX_QBLOCK).transpose(2, 0, 1, 3)
    k_pos = jnp.arange(s)
    scale = HEAD_DIM ** -0.5

    def one_block(args):
        bi, q_b, cum_q = args
        q_pos = bi * FOX_QBLOCK + jnp.arange(FOX_QBLOCK)
        scores = jnp.einsum('bhqd,bhkd->bhqk', q_b, k, preferred_element_type=jnp.float32) * scale
        scores = scores + cum_q[..., :, None] - cum[:, :, None, :]
        scores = jnp.where(k_pos[None, :] <= q_pos[:, None], scores, -jnp.inf)
        p = jax.nn.softmax(scores, axis=-1).astype(v.dtype)
        return jnp.einsum('bhqk,bhkd->bhqd', p, v)

    out = lax.map(one_block, (jnp.arange(n_qb), q_blocks, cum_blocks))
    return out.transpose(1, 2, 0, 3, 4).reshape(b, h, s, dh)


def hybrid_attention_mixer(h, w_in, b_f, w_out):
    b, s, _ = h.shape
    proj = h @ w_in

    def to_heads(t):
        return t.reshape(b, s, N_ATTN_HEADS, HEAD_DIM).transpose(0, 2, 1, 3)

    q = to_heads(proj[..., :ATTN_WIDTH])
    k = to_heads(proj[..., ATTN_WIDTH:2 * ATTN_WIDTH])
    v = to_heads(proj[..., 2 * ATTN_WIDTH:3 * ATTN_WIDTH])
    f_logit = proj[..., 3 * ATTN_WIDTH:] + b_f
    pos = jnp.arange(s)
    q_m = apply_partial_rope(q[:, :N_MOBA_HEADS], pos)
    k_m = apply_partial_rope(k[:, :N_MOBA_HEADS], pos)
    o_moba = moba_attention(q_m, k_m, v[:, :N_MOBA_HEADS])
    log_f = jax.nn.log_sigmoid(f_logit.astype(jnp.float32)).transpose(0, 2, 1)
    o_fox = forgetting_attention(q[:, N_MOBA_HEADS:], k[:, N_MOBA_HEADS:], v[:, N_MOBA_HEADS:], log_f)
    o = jnp.concatenate([o_moba, o_fox], axis=1).transpose(0, 2, 1, 3).reshape(b, s, ATTN_WIDTH)
    return o @ w_out


def _linear_recurrence_combine(left, right):
    a_l, b_l = left
    a_r, b_r = right
    return a_l * a_r, a_r * b_l + b_r


def rglru_mixer(h, w_in, conv_w, conv_b, w_a, b_a, w_x, b_x, lam, w_out):
    b, s, _ = h.shape
    proj = h @ w_in
    gate_branch, u = proj[..., :D_RNN], proj[..., D_RNN:]
    u_pad = jnp.pad(u, ((0, 0), (CONV_WIDTH - 1, 0), (0, 0)))
    conv = conv_b
    for j in range(CONV_WIDTH):
        conv = conv + u_pad[:, j:j + s] * conv_w[j]
    ub = conv.reshape(b, s, N_RNN_BLOCKS, RNN_BLOCK_W)
    r = jax.nn.sigmoid(jnp.einsum('bsnc,ncd->bsnd', ub, w_a).reshape(b, s, D_RNN) + b_a)
    i = jax.nn.sigmoid(jnp.einsum('bsnc,ncd->bsnd', ub, w_x).reshape(b, s, D_RNN) + b_x)
    log_a = (-RG_C * r.astype(jnp.float32)) * jax.nn.softplus(-lam.astype(jnp.float32))
    a = jnp.exp(log_a)
    mult = jnp.sqrt(-jnp.expm1(2.0 * log_a))
    xin = mult * (i * conv).astype(jnp.float32)
    _, hs = lax.associative_scan(_linear_recurrence_combine, (a, xin), axis=1)
    y = jax.nn.gelu(gate_branch) * hs.astype(h.dtype)
    return y @ w_out


def swiglu(h, w_gate, w_up, w_down):
    return (jax.nn.silu(h @ w_gate) * (h @ w_up)) @ w_down


def moe_swiglu(h, w_router, b_router, w_gate, w_up, w_down):
    b, s, d = h.shape
    n_tok = b * s
    xt = h.reshape(n_tok, d)
    logits = (xt @ w_router).astype(jnp.float32) + b_router.astype(jnp.float32)
    top_logit, top_idx = lax.top_k(logits, TOP_K)
    gates = jax.nn.softmax(top_logit, axis=-1)
    n_asg = n_tok * TOP_K
    exp_flat = top_idx.reshape(-1).astype(jnp.int32)
    tok_flat = jnp.repeat(jnp.arange(n_tok, dtype=jnp.int32), TOP_K)
    gate_flat = gates.reshape(-1)
    order = jnp.argsort(exp_flat)
    exp_sorted = exp_flat[order]
    counts = jnp.zeros((N_EXPERTS,), jnp.int32).at[exp_flat].add(1)
    padded = ((counts + MOE_BLOCK - 1) // MOE_BLOCK) * MOE_BLOCK
    starts = jnp.cumsum(counts) - counts
    pends = jnp.cumsum(padded)
    pstarts = pends - padded
    dest = pstarts[exp_sorted] + (jnp.arange(n_asg, dtype=jnp.int32) - starts[exp_sorted])
    n_blocks = -(-n_asg // MOE_BLOCK) + N_EXPERTS
    cap = n_blocks * MOE_BLOCK
    slot_tok = jnp.zeros((cap,), jnp.int32).at[dest].set(tok_flat[order])
    slot_gate = jnp.zeros((cap,), gates.dtype).at[dest].set(gate_flat[order])
    block_starts = jnp.arange(n_blocks, dtype=jnp.int32) * MOE_BLOCK
    block_exp = jnp.minimum(jnp.searchsorted(pends, block_starts, side='right'), N_EXPERTS - 1)

    def one_block(args):
        tok, e = args
        xb = xt[tok]
        return (jax.nn.silu(xb @ w_gate[e]) * (xb @ w_up[e])) @ w_down[e]

    y_slots = lax.map(one_block, (slot_tok.reshape(n_blocks, MOE_BLOCK), block_exp))
    y_slots = y_slots.reshape(cap, d) * slot_gate[:, None].astype(y_slots.dtype)
    y = jax.ops.segment_sum(y_slots, slot_tok, num_segments=n_tok)
    return y.reshape(b, s, d)


def modulated_sublayer(x, fn, shift, scale, gate, g_pre, g_post):
    h = rms_norm(x, g_pre) * (1.0 + scale[:, None, :]) + shift[:, None, :]
    y = rms_norm(fn(h), g_post)
    return x + gate[:, None, :] * y


def setup_inputs(seed: int = 0) -> dict:
    key = jax.random.key(seed)
    ks = jax.random.split(key, 25)
    f32 = jnp.float32
    ne, no = N_EVEN_LAYERS, N_ODD_LAYERS

    def normal(k, shape, fan_in, gain=1.0):
        return jax.random.normal(k, shape, f32) * (gain * fan_in ** -0.5)

    def small(k, shape, s=0.02):
        return s * jax.random.normal(k, shape, f32)

    x = jax.random.normal(ks[0], (BATCH, SEQ, D_MODEL), f32)
    c = jax.random.normal(ks[1], (BATCH, D_MODEL), f32)
    w_ada = normal(ks[2], (DEPTH, D_MODEL, 6 * D_MODEL), D_MODEL, 0.5)
    b_ada = small(ks[3], (DEPTH, 6 * D_MODEL))
    norm_g = 1.0 + 0.05 * jax.random.normal(ks[4], (DEPTH, 4, D_MODEL), f32)
    attn_w_in = normal(ks[5], (ne, D_MODEL, 3 * ATTN_WIDTH + N_FOX_HEADS), D_MODEL)
    fox_b_f = FOX_BIAS_MEAN + 0.5 * jax.random.normal(ks[6], (ne, N_FOX_HEADS), f32)
    attn_w_out = normal(ks[7], (ne, ATTN_WIDTH, D_MODEL), ATTN_WIDTH)
    ffn_w_gate = normal(ks[8], (ne, D_MODEL, D_FF), D_MODEL)
    ffn_w_up = normal(ks[9], (ne, D_MODEL, D_FF), D_MODEL)
    ffn_w_down = normal(ks[10], (ne, D_FF, D_MODEL), D_FF)
    lru_w_in = normal(ks[11], (no, D_MODEL, 2 * D_RNN), D_MODEL)
    lru_conv_w = normal(ks[12], (no, CONV_WIDTH, D_RNN), CONV_WIDTH)
    lru_conv_b = small(ks[13], (no, D_RNN))
    lru_w_a = normal(ks[14], (no, N_RNN_BLOCKS, RNN_BLOCK_W, RNN_BLOCK_W), RNN_BLOCK_W)
    lru_b_a = small(ks[15], (no, D_RNN))
    lru_w_x = normal(ks[16], (no, N_RNN_BLOCKS, RNN_BLOCK_W, RNN_BLOCK_W), RNN_BLOCK_W)
    lru_b_x = small(ks[17], (no, D_RNN))
    a_pow = jax.random.uniform(ks[18], (no, D_RNN), f32, minval=0.9, maxval=0.999)
    a_base = a_pow ** (1.0 / RG_C)
    lru_lambda = jnp.log(a_base) - jnp.log1p(-a_base)
    lru_w_out = normal(ks[19], (no, D_RNN, D_MODEL), D_RNN)
    moe_w_router = normal(ks[20], (no, D_MODEL, N_EXPERTS), D_MODEL)
    moe_b_router = small(ks[21], (no, N_EXPERTS), 0.01)
    moe_w_gate = normal(ks[22], (no, N_EXPERTS, D_MODEL, D_EXPERT), D_MODEL)
    moe_w_up = normal(ks[23], (no, N_EXPERTS, D_MODEL, D_EXPERT), D_MODEL)
    moe_w_down = normal(ks[24], (no, N_EXPERTS, D_EXPERT, D_MODEL), D_EXPERT)
    return {"x": x, "c": c, "w_ada": w_ada, "b_ada": b_ada, "norm_g": norm_g,
            "attn_w_in": attn_w_in, "fox_b_f": fox_b_f, "attn_w_out": attn_w_out,
            "ffn_w_gate": ffn_w_gate, "ffn_w_up": ffn_w_up, "ffn_w_down": ffn_w_down,
            "lru_w_in": lru_w_in, "lru_conv_w": lru_conv_w, "lru_conv_b": lru_conv_b,
            "lru_w_a": lru_w_a, "lru_b_a": lru_b_a, "lru_w_x": lru_w_x, "lru_b_x": lru_b_x,
            "lru_lambda": lru_lambda, "lru_w_out": lru_w_out,
            "moe_w_router": moe_w_router, "moe_b_router": moe_b_router,
            "moe_w_gate": moe_w_gate, "moe_w_up": moe_w_up, "moe_w_down": moe_w_down}


def reference(x, c, w_ada, b_ada, norm_g, attn_w_in, fox_b_f, attn_w_out,
              ffn_w_gate, ffn_w_up, ffn_w_down, lru_w_in, lru_conv_w, lru_conv_b,
              lru_w_a, lru_b_a, lru_w_x, lru_b_x, lru_lambda, lru_w_out,
              moe_w_router, moe_b_router, moe_w_gate, moe_w_up, moe_w_down):
    cond = jax.nn.silu(c)
    for layer in range(DEPTH):
        i = layer // 2
        mod = cond @ w_ada[layer] + b_ada[layer]
        sh_m, sc_m, g_m, sh_f, sc_f, g_f = jnp.split(mod, 6, axis=-1)
        if layer % 2 == 0:
            mixer = functools.partial(hybrid_attention_mixer, w_in=attn_w_in[i], b_f=fox_b_f[i],
                                      w_out=attn_w_out[i])
            ffn = functools.partial(swiglu, w_gate=ffn_w_gate[i], w_up=ffn_w_up[i], w_down=ffn_w_down[i])
        else:
            mixer = functools.partial(rglru_mixer, w_in=lru_w_in[i], conv_w=lru_conv_w[i],
                                      conv_b=lru_conv_b[i], w_a=lru_w_a[i], b_a=lru_b_a[i],
                                      w_x=lru_w_x[i], b_x=lru_b_x[i], lam=lru_lambda[i],
                                      w_out=lru_w_out[i])
            ffn = functools.partial(moe_swiglu, w_router=moe_w_router[i], b_router=moe_b_router[i],
                                    w_gate=moe_w_gate[i], w_up=moe_w_up[i], w_down=moe_w_down[i])
        x = modulated_sublayer(x, mixer, sh_m, sc_m, g_m, norm_g[layer, 0], norm_g[layer, 1])
        x = modulated_sublayer(x, ffn, sh_f, sc_f, g_f, norm_g[layer, 2], norm_g[layer, 3])
    return x
```

```python
import os
from contextlib import ExitStack
import numpy as np
import ml_dtypes
import concourse.bass as bass
import concourse.mybir as mybir
from concourse.bass_utils import run_bass_kernel_spmd

F32 = mybir.dt.float32
BF16 = mybir.dt.bfloat16
I32 = mybir.dt.int32
AF = mybir.ActivationFunctionType
ALU = mybir.AluOpType
AX = mybir.AxisListType

D = 1024
SEQ = 4096
NT = SEQ // 128
NEXP = int(os.environ.get('K_NEXP', '8'))
EPS = 1e-6
ENGS = ["pe", "act", "dve", "pool", "sp"]


class _Op:
    __slots__ = ("eng", "fn", "deps", "dma", "signal", "sig")

    def __init__(self, eng, fn, dma):
        self.eng = eng
        self.fn = fn
        self.deps = []
        self.dma = dma
        self.signal = False
        self.sig = None


class Sched:
    NDMA = 28

    def __init__(self, nc, es):
        self.nc = nc
        self.e = {"pe": nc.tensor, "act": nc.scalar, "dve": nc.vector, "pool": nc.gpsimd, "sp": nc.sync}
        self.sem = {k: es.enter_context(nc.semaphore("sem_" + k)) for k in ENGS}
        self.dsem = [es.enter_context(nc.semaphore("dsem%d" % i)) for i in range(self.NDMA)]
        self.bsem = es.enter_context(nc.semaphore("bsem"))
        self.cnt = {k: 0 for k in ENGS}
        self.dtot = [0] * self.NDMA
        self.dnext = 0
        self.NSW = 8
        self.dnext_sw = 0
        self.btot = 0
        self.waited = {k: {} for k in ENGS}
        self.ops = []
        self.lastw = {}
        self.readers = {}
        self.nemit = 0
        self._cap = []
        self.bar_src = None
        self.bar_dst = None

    def _rec(self, eng, fn, r, w, dma):
        op = _Op(eng, fn, dma)
        deps = []
        for k in r:
            lw = self.lastw.get(k)
            if lw is not None:
                deps.append(lw)
            self.readers.setdefault(k, []).append(op)
        for k in w:
            lw = self.lastw.get(k)
            if lw is not None:
                deps.append(lw)
            for rd in self.readers.get(k, ()):
                if rd is not op:
                    deps.append(rd)
            self.lastw[k] = op
            self.readers[k] = []
        seen = set()
        for d in deps:
            if id(d) in seen or d is op:
                continue
            seen.add(id(d))
            if (not d.dma) and (not dma) and d.eng == "pe" and eng == "pe":
                continue
            d.signal = True
            op.deps.append(d)
        self.ops.append(op)
        return op

    def op(self, eng, fn, r=(), w=()):
        if self._cap:
            self._cap[-1].append((eng, fn, tuple(r), tuple(w), False))
            return None
        return self._rec(eng, fn, r, w, False)

    def dma(self, q, fn, r=(), w=()):
        if self._cap:
            self._cap[-1].append((q, fn, tuple(r), tuple(w), True))
            return None
        return self._rec(q, fn, r, w, True)

    def capture(self, body):
        self._cap.append([])
        try:
            body()
        finally:
            ops = self._cap.pop()
        return ops

    def replay(self, threads):
        threads = [t for t in threads if t]
        if not threads:
            return
        n = max(len(t) for t in threads)
        pos = [0] * len(threads)
        for step in range(1, n + 1):
            for i, t in enumerate(threads):
                tgt = (step * len(t)) // n
                while pos[i] < tgt:
                    if self._cap:
                        self._cap[-1].append(t[pos[i]])
                    else:
                        self._rec(*t[pos[i]])
                    pos[i] += 1

    def _wait(self, eng, sem, val):
        wd = self.waited[eng]
        key = id(sem)
        if wd.get(key, 0) >= val:
            return
        wd[key] = val
        self.e[eng].wait_ge(sem, val)

    def flush(self, barrier=True):
        ops = self.ops
        if barrier:
            last = {}
            for op in ops:
                if not op.dma:
                    last[op.eng] = op
            for op in last.values():
                op.signal = True
        for op in ops:
            for d in op.deps:
                sem, val = d.sig
                self._wait(op.eng, sem, val)
            if op.dma:
                if op.eng == "pool":
                    slot = self.NDMA - self.NSW + self.dnext_sw
                    self.dnext_sw = (self.dnext_sw + 1) % self.NSW
                else:
                    slot = self.dnext
                    self.dnext = (self.dnext + 1) % (self.NDMA - self.NSW)
                if self.dtot[slot] > 0:
                    self._wait(op.eng, self.dsem[slot], self.dtot[slot])
                inst = op.fn()
                self.dtot[slot] += 16
                inst.then_inc(self.dsem[slot], 16)
                op.sig = (self.dsem[slot], self.dtot[slot])
            else:
                inst = op.fn()
                if op.signal:
                    self.cnt[op.eng] += 1
                    inst.then_inc(self.sem[op.eng], 1)
                    op.sig = (self.sem[op.eng], self.cnt[op.eng])
            self.nemit += 1
        self.ops = []
        self.lastw = {}
        self.readers = {}
        if barrier:
            for k in ENGS:
                if k != "sp" and self.cnt[k] > 0:
                    self._wait("sp", self.sem[k], self.cnt[k])
            for s in range(self.NDMA):
                if self.dtot[s] > 0:
                    self._wait("sp", self.dsem[s], self.dtot[s])
            self.btot += 16
            self.nc.sync.dma_start(out=self.bar_dst, in_=self.bar_src).then_inc(self.bsem, 16)
            for k in ENGS:
                self.e[k].wait_ge(self.bsem, self.btot)


def stage0(S, nc, es, T, P):
    st = ExitStack()
    sb = lambda name, shape, dt=F32: st.enter_context(nc.sbuf_tensor(name, shape, dt))
    ps = lambda name, shape, dt=F32: st.enter_context(nc.psum_tensor(name, shape, dt))
    one11 = sb("s0_one", [1, 1])
    ones_row = sb("s0_onesrow", [1, 128])
    ones128 = sb("s0_ones128", [128, 128])
    crow = sb("s0_crow", [1, D])
    grow = sb("s0_grow", [1, 8 * D])
    brow = sb("s0_brow", [1, 2 * 6 * D])
    condT = sb("s0_condT", [128, 8])
    cond_rep = sb("s0_condrep", [128, 8, 128], BF16)
    condTb = sb("s0_condTb", [128, 8], BF16)
    gT = sb("s0_gT", [128, 64])
    modT = sb("s0_modT", [128, 64])
    gpost = sb("s0_gpost", [128, 4, D])
    wsl = [sb("s0_w%d" % i, [128, 8, 512], BF16) for i in range(3)]
    pT = ps("s0_pT", [128, 512])
    pG = ps("s0_pG", [128, 512])
    pM = ps("s0_pM", [128, 512])
    pR = [ps("s0_pR%d" % i, [128, 512]) for i in range(2)]
    V, A, PE_, PO = nc.vector, nc.scalar, nc.tensor, nc.gpsimd

    S.op("pool", lambda: PO.memset(one11[:], 1.0), w=["one11"])
    S.op("pool", lambda: PO.memset(ones_row[:], 1.0), w=["ones_row"])
    S.op("pool", lambda: PO.memset(ones128[:], 1.0), w=["ones128"])
    S.dma("sp", lambda: nc.sync.dma_start(out=crow[:], in_=T["c"]), w=["crow"])
    S.dma("sp", lambda: nc.sync.dma_start(out=grow[:], in_=T["norm_g"]), w=["grow"])
    S.dma("sp", lambda: nc.sync.dma_start(out=brow[:], in_=T["b_ada"]), w=["brow"])
    for sub in range(4):
        L, which = sub // 2, sub % 2
        S.dma("sp", lambda sub=sub, L=L, which=which: nc.sync.dma_start(
            out=gpost[:, sub, :], in_=T["norm_g"][0:1, (L * 4 + 2 * which + 1) * D:(L * 4 + 2 * which + 2) * D].partition_broadcast(128)),
            w=["gpost%d" % sub])
    for kc in range(8):
        S.op("pe", lambda kc=kc: PE_.matmul(pT[:, kc:kc + 1], lhsT=crow[0:1, kc * 128:(kc + 1) * 128], rhs=one11[0:1, 0:1],
                                            start=True, stop=True), r=["crow", "one11"], w=["pT"])
    S.op("act", lambda: A.activation(out=condT[:], in_=pT[:, 0:8], func=AF.Silu), r=["pT"], w=["condT"])
    S.op("dve", lambda: V.tensor_copy(out=condTb[:], in_=condT[:]), r=["condT"], w=["condTb"])
    for kc in range(8):
        S.op("dve", lambda kc=kc: V.tensor_scalar(out=cond_rep[:, kc, :], in0=ones128[:], scalar1=condT[:, kc:kc + 1], scalar2=None,
                                                  op0=ALU.mult), r=["condT", "ones128"], w=["cond_rep"])
    for j in range(64):
        S.op("pe", lambda j=j: PE_.matmul(pG[:, j:j + 1], lhsT=grow[0:1, j * 128:(j + 1) * 128], rhs=one11[0:1, 0:1],
                                          start=True, stop=True), r=["grow", "one11"], w=["pG"])
    S.op("dve", lambda: V.tensor_copy(out=gT[:], in_=pG[:, 0:64]), r=["pG"], w=["gT"])
    w_ada = T["w_ada"]
    tidx = {0: 0, 1: 1, 3: 2, 4: 3}
    ri = 0
    for L in range(2):
        for s in range(12):
            buf = (L * 12 + s) % 3
            grp, half = s // 2, s % 2
            S.dma("pool", lambda L=L, s=s, buf=buf: PO.dma_start(
                out=wsl[buf][:], in_=w_ada[L, :, s * 512:(s + 1) * 512].rearrange("(kc p) n -> p kc n", p=128)),
                w=["wsl%d" % buf])
            boff = L * 6 * D + s * 512
            if grp in (2, 5):
                pr = pR[ri % 2]
                prk = "pR%d" % (ri % 2)
                ri += 1
                for kc in range(8):
                    S.op("pe", lambda kc=kc, buf=buf, pr=pr: PE_.matmul(pr[:, :], lhsT=cond_rep[:, kc, :], rhs=wsl[buf][:, kc, :],
                                                                    start=(kc == 0), stop=False),
                         r=["cond_rep", "wsl%d" % buf], w=[prk])
                S.op("pe", lambda boff=boff, pr=pr: PE_.matmul(pr[:, :], lhsT=ones_row[0:1, :], rhs=brow[0:1, boff:boff + 512],
                                                             start=False, stop=True), r=["ones_row", "brow"], w=[prk])
                sub = 2 * L + (0 if grp == 2 else 1)
                S.op("dve", lambda sub=sub, half=half, pr=pr: V.tensor_tensor(
                    out=P["Brep"][:, sub, half * 512:(half + 1) * 512], in0=pr[:, :], in1=gpost[:, sub, half * 512:(half + 1) * 512],
                    op=ALU.mult), r=[prk, "gpost%d" % sub], w=["Brep"])
            else:
                for q in range(4):
                    col = L * 32 + tidx[grp] * 8 + half * 4 + q
                    for kc in range(8):
                        S.op("pe", lambda kc=kc, buf=buf, q=q, col=col: PE_.matmul(
                            pM[:, col:col + 1], lhsT=wsl[buf][:, kc, q * 128:(q + 1) * 128], rhs=condTb[:, kc:kc + 1],
                            start=(kc == 0), stop=False), r=["condTb", "wsl%d" % buf], w=["pM"])
                    S.op("pe", lambda boff=boff, q=q, col=col: PE_.matmul(
                        pM[:, col:col + 1], lhsT=brow[0:1, boff + q * 128:boff + (q + 1) * 128], rhs=one11[0:1, 0:1],
                        start=False, stop=True), r=["brow", "one11"], w=["pM"])
    S.op("dve", lambda: V.tensor_copy(out=modT[:], in_=pM[:, 0:64]), r=["pM"], w=["modT"])
    for sub in range(4):
        L, which = sub // 2, sub % 2
        sh_c = L * 32 + (0 if which == 0 else 2) * 8
        sc_c = L * 32 + (1 if which == 0 else 3) * 8
        g_c = (L * 4 + 2 * which) * 8
        S.op("dve", lambda sub=sub, sc_c=sc_c, g_c=g_c: V.scalar_tensor_tensor(
            out=P["AT"][:, sub, :], in0=modT[:, sc_c:sc_c + 8], scalar=1.0, in1=gT[:, g_c:g_c + 8], op0=ALU.add, op1=ALU.mult),
            r=["modT", "gT"], w=["AT"])
        S.op("dve", lambda sub=sub, sh_c=sh_c: V.tensor_copy(out=P["shT"][:, sub, :], in_=modT[:, sh_c:sh_c + 8]),
             r=["modT"], w=["shT"])
    S.flush()
    st.close()


class Ring:
    def __init__(self, name, tens):
        self.name, self.tens, self.i = name, tens, 0

    def next(self):
        t = self.tens[self.i % len(self.tens)]
        k = "%s%d" % (self.name, self.i % len(self.tens))
        self.i += 1
        return t, k


def mk(nc, st):
    sb = lambda name, shape, dt=F32: st.enter_context(nc.sbuf_tensor(name, shape, dt))
    ps = lambda name, shape, dt=F32: st.enter_context(nc.psum_tensor(name, shape, dt))
    return sb, ps


def prenorm_tile(S, nc, W, xt, kx, sub, P, C, dst_fn, kdst, want32=None, k32=None):
    V, A, PE_ = nc.vector, nc.scalar, nc.tensor
    s_ = W["i"] % len(W["junk"])
    W["i"] += 1
    junk, ss, rstd, xn = W["junk"][s_], W["ss"][s_], W["rstd"][s_], W["xn"][s_]
    tp = W["tp"][s_ % len(W["tp"])]
    ktp = "pn_tp%d" % (s_ % len(W["tp"]))
    p_ = "pn%d_" % s_
    S.op("act", lambda: A.activation(out=junk[:], in_=xt, func=AF.Square, accum_out=ss[:, 0:1]),
         r=[kx], w=[p_ + "junk", p_ + "ss"])
    S.op("act", lambda: A.activation(out=ss[:, 2:3], in_=ss[:, 0:1], func=AF.Sqrt, scale=1.0 / D, bias=EPS),
         r=[p_ + "ss"], w=[p_ + "ss3"])
    S.op("dve", lambda: V.reciprocal(out=rstd[:, 0:1], in_=ss[:, 2:3]), r=[p_ + "ss3"], w=[p_ + "rstd"])
    S.op("act", lambda: A.activation(out=xn[:], in_=xt, func=AF.Copy, scale=rstd[:, 0:1]),
         r=[kx, p_ + "rstd"], w=[p_ + "xn"])
    for kc in range(8):
        S.op("pe", lambda kc=kc: PE_.transpose(out=tp[:, kc * 128:(kc + 1) * 128], in_=xn[:, kc * 128:(kc + 1) * 128],
                                              identity=C["ident"][:]), r=[p_ + "xn", "ident"], w=[ktp])
    for kc in range(8):
        if want32 is not None:
            S.op("dve", lambda kc=kc: V.tensor_scalar(out=want32(kc), in0=tp[:, kc * 128:(kc + 1) * 128],
                                                      scalar1=P["AT"][:, sub, kc:kc + 1], scalar2=P["shT"][:, sub, kc:kc + 1],
                                                      op0=ALU.mult, op1=ALU.add), r=[ktp, "AT", "shT"], w=[k32 + "_%d" % kc])
            S.op("pool", lambda kc=kc: nc.gpsimd.tensor_copy(out=dst_fn(kc), in_=want32(kc)), r=[k32 + "_%d" % kc], w=[kdst + "_%d" % kc])
        else:
            S.op("dve", lambda kc=kc: V.tensor_scalar(out=dst_fn(kc), in0=tp[:, kc * 128:(kc + 1) * 128],
                                                      scalar1=P["AT"][:, sub, kc:kc + 1], scalar2=P["shT"][:, sub, kc:kc + 1],
                                                      op0=ALU.mult, op1=ALU.add), r=[ktp, "AT", "shT"], w=[kdst + "_%d" % kc])


def prenorm_work(nc, st, pfx, ntp=1, nset=2):
    sb, ps = mk(nc, st)
    return {"i": 0, "tp": [ps(pfx + "tp%d" % i, [128, D]) for i in range(ntp)], "junk": [sb(pfx + "junk%d" % i, [128, D], BF16) for i in range(nset)],
            "ss": [sb(pfx + "ss%d" % i, [128, 4]) for i in range(nset)], "rstd": [sb(pfx + "rstd%d" % i, [128, 1]) for i in range(nset)],
            "xn": [sb(pfx + "xn%d" % i, [128, D]) for i in range(nset)]}


def postnorm_tile(S, nc, W, ysrc, ky, xin, kxin, xout, kxout, sub, P):
    V, A = nc.vector, nc.scalar
    s_ = W["i"] % len(W["junk"])
    W["i"] += 1
    junk, ss, rstd, tmp = W["junk"][s_], W["ss"][s_], W["rstd"][s_], W["tmp"][s_]
    p_ = "po%d_" % s_
    for hf in range(2):
        S.op("act", lambda hf=hf: A.activation(out=junk[:, hf * 512:(hf + 1) * 512], in_=ysrc(hf), func=AF.Square,
                                               accum_out=ss[:, hf:hf + 1]), r=ky, w=[p_ + "junk%d" % hf, p_ + "ss%d" % hf])
    S.op("dve", lambda: V.tensor_tensor(out=ss[:, 2:3], in0=ss[:, 0:1], in1=ss[:, 1:2], op=ALU.add),
         r=[p_ + "ss0", p_ + "ss1"], w=[p_ + "s2"])
    S.op("act", lambda: A.activation(out=ss[:, 4:5], in_=ss[:, 2:3], func=AF.Sqrt, scale=1.0 / D, bias=EPS),
         r=[p_ + "s2"], w=[p_ + "s4"])
    S.op("dve", lambda: V.reciprocal(out=rstd[:, 0:1], in_=ss[:, 4:5]), r=[p_ + "s4"], w=[p_ + "rstd"])
    for hf in range(2):
        S.op("dve", lambda hf=hf: V.scalar_tensor_tensor(out=tmp[:, hf * 512:(hf + 1) * 512], in0=ysrc(hf), scalar=rstd[:, 0:1],
                                                         in1=P["Brep"][:, sub, hf * 512:(hf + 1) * 512], op0=ALU.mult, op1=ALU.mult),
             r=ky + [p_ + "rstd", "Brep"], w=[p_ + "tmp%d" % hf])
        S.op("pool", lambda hf=hf: nc.gpsimd.tensor_tensor(out=xout[:, hf * 512:(hf + 1) * 512], in0=tmp[:, hf * 512:(hf + 1) * 512],
                                                            in1=xin[:, hf * 512:(hf + 1) * 512], op=ALU.add),
             r=[p_ + "tmp%d" % hf, kxin], w=[kxout])


def postnorm_work(nc, st, pfx, nset=2):
    sb, ps = mk(nc, st)
    return {"i": 0, "junk": [sb(pfx + "junk%d" % i, [128, D], BF16) for i in range(nset)],
            "ss": [sb(pfx + "ss%d" % i, [128, 8]) for i in range(nset)], "rstd": [sb(pfx + "rstd%d" % i, [128, 1]) for i in range(nset)],
            "tmp": [sb(pfx + "tmp%d" % i, [128, D]) for i in range(nset)]}


def k8(k):
    return [k + "_%d" % i for i in range(8)]

def stage1(S, nc, T, P, C, R):
    st = ExitStack()
    sb, ps = mk(nc, st)
    V, A, PE_, PO = nc.vector, nc.scalar, nc.tensor, nc.gpsimd
    w_bf = sb("s1_w", [128, 8, 3080], BF16)
    CTt = sb("s1_CT", [128, SEQ])
    STt = sb("s1_ST", [128, SEQ])
    msw = sb("s1_msw", [128, 128], BF16)
    U = sb("s1_U", [128, 128])
    ones128 = sb("s1_ones", [128, 128])
    bfr = sb("s1_bfr", [128, 8])
    gmask = sb("s1_gmask", [128, 16, 128])
    xring = Ring("s1x", [sb("s1_x%d" % i, [128, D]) for i in range(2)])
    hring = Ring("s1h", [sb("s1_h%d" % i, [128, 8, 512], BF16) for i in range(2)])
    Wn = prenorm_work(nc, st, "s1n_")
    qbring = Ring("s1qb", [sb("s1_qb%d" % i, [128, 512], BF16) for i in range(2)])
    qoring = Ring("s1qo", [sb("s1_qo%d" % i, [128, 512], BF16) for i in range(3)])
    t1 = sb("s1_t1", [128, 512])
    t2 = sb("s1_t2", [128, 512])
    qmring = Ring("s1qm", [sb("s1_qm%d" % i, [128, 4, 512], BF16) for i in range(2)])
    ksum = sb("s1_ksum", [128, 4, 16])
    ksum_bf = sb("s1_ksumbf", [128, 4, 32], BF16)
    vring = Ring("s1v", [sb("s1_v%d" % i, [128, 16, 65], BF16) for i in range(2)])
    PL = sb("s1_PL", [128, 32, 8])
    zt = sb("s1_zt", [128, 8])
    ez = sb("s1_ez", [128, 8])
    accs = sb("s1_accs", [128, 8])
    cumpos = sb("s1_cum", [128, 32, 8])
    CG = sb("s1_CG", [128, 8, 8])
    rqt = sb("s1_rqt", [128, 8])
    NB2 = sb("s1_NB2", [128, 8, 32, 8])
    GB = sb("s1_GB", [128, 8, 16])
    top8 = sb("s1_top8", [128, 8, 8])
    sel = sb("s1_sel", [128, 8, 16])
    nsel = sb("s1_nsel", [128, 8, 16], BF16)
    nring = Ring("s1ns", [sb("s1_ns%d" % i, [16, 8, 512], BF16) for i in range(2)])
    rqst = sb("s1_rq", [8, SEQ], BF16)
    pjring = Ring("s1pj", [ps("s1_pj%d" % i, [128, 512]) for i in range(2)])
    psw = ps("s1_psw", [128, 512])
    pm = ps("s1_pm", [128, 512])
    ptr = ps("s1_ptr", [16, 1024], BF16)
    pc = ps("s1_pc", [128, 512])

    w_in = T["attn_w_in"]
    for i in range(4):
        S.dma("pool", lambda i=i: PO.dma_start(out=w_bf[:, :, i * 770:(i + 1) * 770],
                                               in_=w_in[:, i * 770:(i + 1) * 770].rearrange("(kc p) n -> p kc n", p=128)), w=["w_bf"])
    S.dma("sp", lambda: nc.sync.dma_start(out=CTt[:], in_=T["ropeC"]), w=["CT"])
    S.dma("sp", lambda: nc.sync.dma_start(out=STt[:], in_=T["ropeS"]), w=["ST"])
    S.dma("pool", lambda: PO.dma_start(out=msw[:], in_=T["msw"]), w=["msw"])
    S.dma("sp", lambda: nc.sync.dma_start(out=U[:], in_=T["utri"]), w=["U"])
    S.dma("sp", lambda: nc.sync.dma_start(out=bfr[:], in_=T["fox_b_f"].partition_broadcast(128)), w=["bfr"])
    S.dma("sp", lambda: nc.sync.dma_start(out=gmask[:].rearrange("p a b -> p (a b)"), in_=T["gmask"]), w=["gmask"])
    S.op("pool", lambda: PO.memset(ones128[:], 1.0), w=["ones128"])
    S.op("pool", lambda: PO.memset(accs[:], 0.0), w=["accs"])
    S.op("pool", lambda: PO.memset(NB2[:], 0.0), w=["NB2"])
    S.op("pool", lambda: PO.memset(ksum[:], 0.0), w=["ksum"])
    S.op("pool", lambda: PO.memset(ksum_bf[:], 0.0), w=["ksum_bf"])
    for i in range(2):
        S.op("pool", lambda i=i: PO.memset(vring.tens[i][:], 1.0), w=["s1v%d" % i])
    x = T["x"]

    def rope(pj, kp, scale, g, dest, kdest):
        qb, kqb = qbring.next()
        S.op("act", lambda: A.activation(out=qb[:], in_=pj[:], func=AF.Copy, scale=scale), r=[kp], w=[kqb])
        S.op("pe", lambda: PE_.matmul(psw[:], lhsT=msw[:], rhs=qb[:], start=True, stop=True), r=[kqb, "msw"], w=["psw"])
        S.op("dve", lambda: V.tensor_tensor(out=t1[:], in0=qb[:], in1=CTt[:, g * 512:(g + 1) * 512], op=ALU.mult),
             r=[kqb, "CT"], w=["t1"])
        S.op("dve", lambda: V.tensor_tensor(out=t2[:], in0=psw[:], in1=STt[:, g * 512:(g + 1) * 512], op=ALU.mult),
             r=["psw", "ST"], w=["t2"])
        S.op("pool", lambda: PO.tensor_tensor(out=dest, in0=t1[:], in1=t2[:], op=ALU.add), r=["t1", "t2"], w=[kdest])

    SKIP = os.environ.get("S1_SKIP", "")
    NG = int(os.environ.get("S1_NG", "8"))
    ctx = {}

    def pn(g):
        hT, kh = hring.next()
        hkeys = []
        for t in range(4):
            ti = 4 * g + t
            xt, kx = xring.next()
            S.dma("sp", lambda xt=xt, ti=ti: nc.sync.dma_start(out=xt[:], in_=x[ti * 128:(ti + 1) * 128, :]), w=[kx])
            kd = "%st%d" % (kh, t)
            prenorm_tile(S, nc, Wn, xt[:], kx, 0, P, C, lambda kc, hT=hT, t=t: hT[:, kc, t * 128:(t + 1) * 128], kd)
            hkeys += k8(kd)
        ctx[g] = (hT, hkeys)

    def main(g):
        hT, hkeys = ctx[g]
        qm, kqm = qmring.next()
        km, kkm = qmring.next()
        for isk in range(0 if "Q" in SKIP else 2):
            for j in range(8):
                pj, kp = pjring.next()
                c0 = isk * 1024 + j * 128
                for kc in range(8):
                    S.op("pe", lambda pj=pj, kc=kc, c0=c0, hT=hT: PE_.matmul(pj[:], lhsT=w_bf[:, kc, c0:c0 + 128], rhs=hT[:, kc, :],
                                                                         start=(kc == 0), stop=(kc == 7)), r=["w_bf"] + hkeys, w=[kp])
                scale = 1.0 if isk else 0.125
                dst_d = (R["KT"] if isk else R["QT"])[j, :, g * 512:(g + 1) * 512]
                if j >= 4 or "R" in SKIP:
                    qo, kq = qoring.next()
                    S.op("act", lambda qo=qo, pj=pj, scale=scale: A.activation(out=qo[:], in_=pj[:], func=AF.Copy, scale=scale),
                         r=[kp], w=[kq])
                    S.dma("sp", lambda qo=qo, dst_d=dst_d: nc.sync.dma_start(out=dst_d, in_=qo[:]), r=[kq])
                else:
                    buf, kb = (km, kkm) if isk else (qm, kqm)
                    kdst = "%s_j%d" % (kb, j)
                    rope(pj, kp, scale, g, buf[:, j, :], kdst)
                    S.dma("sp", lambda buf=buf, j=j, dst_d=dst_d: nc.sync.dma_start(out=dst_d, in_=buf[:, j, :]), r=[kdst])
                    if isk:
                        S.op("dve", lambda buf=buf, j=j, g=g: V.tensor_reduce(
                            out=ksum[:, j, 2 * g:2 * g + 2], in_=buf[:, j, :].rearrange("p (b t) -> p b t", t=256),
                            axis=AX.X, op=ALU.add), r=[kdst], w=["ksum"])
        S.op("pool", lambda: PO.tensor_copy(out=ksum_bf[0:64, :, 0:16], in_=ksum[0:64, :, :]), r=["ksum"], w=["ksum_bf"])
        S.op("pool", lambda: PO.tensor_copy(out=ksum_bf[64:128, :, 16:32], in_=ksum[64:128, :, :]), r=["ksum"], w=["ksum_bf"])
        for t in range(0 if "V" in SKIP else 4):
            ti = 4 * g + t
            vs, kv = vring.next()
            for hf in range(2):
                pj, kp = pjring.next()
                for kc in range(8):
                    S.op("pe", lambda pj=pj, kc=kc, hf=hf, hT=hT, t=t: PE_.matmul(
                        pj[:], lhsT=hT[:, kc, t * 128:(t + 1) * 128], rhs=w_bf[:, kc, 2048 + hf * 512:2048 + (hf + 1) * 512],
                        start=(kc == 0), stop=(kc == 7)), r=["w_bf"] + hkeys, w=[kp])
                eng = "act" if hf == 0 else "dve"
                if hf == 0:
                    S.op("act", lambda pj=pj, vs=vs, hf=hf: A.activation(out=vs[:, hf * 8:(hf + 1) * 8, 0:64],
                                                                         in_=pj[:].rearrange("p (h d) -> p h d", d=64), func=AF.Copy),
                         r=[kp], w=[kv + "_h%d" % hf])
                else:
                    S.op("dve", lambda pj=pj, vs=vs, hf=hf: V.tensor_copy(out=vs[:, hf * 8:(hf + 1) * 8, 0:64],
                                                                          in_=pj[:].rearrange("p (h d) -> p h d", d=64)),
                         r=[kp], w=[kv + "_h%d" % hf])
            S.dma("sp", lambda vs=vs, ti=ti: nc.sync.dma_start(out=R["V"][ti], in_=vs[:].rearrange("p h d -> p (h d)")),
                  r=[kv + "_h0", kv + "_h1"], w=[kv])
            for kc in range(8):
                S.op("pe", lambda kc=kc, hT=hT, t=t: PE_.matmul(pm[:, t * 8:(t + 1) * 8], lhsT=hT[:, kc, t * 128:(t + 1) * 128],
                                                               rhs=w_bf[:, kc, 3072:3080], start=(kc == 0), stop=(kc == 7)),
                     r=["w_bf"] + hkeys, w=["pm"])
            S.op("dve", lambda t=t: V.tensor_tensor(out=zt[:], in0=pm[:, t * 8:(t + 1) * 8], in1=bfr[:], op=ALU.add),
                 r=["pm", "bfr"], w=["zt"])
            S.op("act", lambda: A.activation(out=ez[:], in_=zt[:], func=AF.Exp, scale=-1.0), r=["zt"], w=["ez"])
            S.op("act", lambda ti=ti: A.activation(out=PL[:, ti, :], in_=ez[:], func=AF.Ln, bias=1.0), r=["ez"], w=["PL"])
        ns, kns = nring.next()
        if "G" in SKIP:
            return
        for t in range(4):
            ti = 4 * g + t
            nv = ti // 2
            for j in range(4):
                S.op("pe", lambda j=j, t=t, qm=qm: PE_.matmul(
                    pm[:, 64 + j * 32:64 + j * 32 + 32], lhsT=qm[:, j, t * 128:(t + 1) * 128],
                    rhs=ksum_bf[:, j, :], start=True, stop=True),
                    r=["ksum_bf", "%s_j%d" % (kqm, j)], w=["pm"])
            S.op("dve", lambda nv=nv: V.tensor_tensor(out=GB[:].rearrange("p h n -> p (h n)"), in0=pm[:, 64:192],
                                                      in1=gmask[:, nv, :], op=ALU.add), r=["pm", "gmask"], w=["GB"])
            for h in range(8):
                S.op("dve", lambda h=h: V.max(out=top8[:, h, :], in_=GB[:, h, :]), r=["GB"], w=["top8"])
            for h in range(8):
                S.op("dve", lambda h=h: V.tensor_scalar(out=sel[:, h, :], in0=GB[:, h, :], scalar1=top8[:, h, 2:3], scalar2=None,
                                                        op0=ALU.is_ge), r=["GB", "top8"], w=["sel"])
            S.op("dve", lambda nv=nv: V.memset(sel[:, :, nv:nv + 1], 1.0), w=["sel"])
            S.op("dve", lambda: V.tensor_scalar(out=nsel[:], in0=sel[:], scalar1=-1.0, scalar2=30000.0, op0=ALU.add, op1=ALU.mult),
                 r=["sel"], w=["nsel"])
            for h in range(8):
                S.op("pe", lambda h=h: PE_.transpose(out=ptr[0:16, h * 128:(h + 1) * 128], in_=nsel[:, h, :], identity=C["identb"][:]),
                     r=["nsel", "identb"], w=["ptr"])
            S.op("act", lambda ns=ns, t=t: A.activation(out=ns[:, :, t * 128:(t + 1) * 128],
                                                        in_=ptr[0:16, :].rearrange("p (h q) -> p h q", q=128), func=AF.Copy),
                 r=["ptr"], w=[kns])
        S.dma("sp", lambda ns=ns, g=g: nc.sync.dma_start(out=R["NS"][:, :, g * 512:(g + 1) * 512], in_=ns[:]), r=[kns])
    def cum(G):
        for ti in range(4 * G, 4 * G + 4):
            if ti % 4 == 0:
                S.op("pe", lambda: PE_.matmul(pc[:, 8:16], lhsT=ones128[:], rhs=accs[:], start=True, stop=True),
                     r=["ones128", "accs"], w=["pc"])
                S.op("dve", lambda: V.tensor_copy(out=CG[:, G, :], in_=pc[:, 8:16]), r=["pc"], w=["CG"])
            S.op("pe", lambda ti=ti: PE_.matmul(pc[:, 0:8], lhsT=U[:], rhs=PL[:, ti, :], start=True, stop=False),
                 r=["U", "PL"], w=["pc"])
            S.op("pe", lambda: PE_.matmul(pc[:, 0:8], lhsT=ones128[:], rhs=accs[:], start=False, stop=True),
                 r=["ones128", "accs"], w=["pc"])
            S.op("dve", lambda ti=ti: V.tensor_copy(out=cumpos[:, ti, :], in_=pc[:, 0:8]), r=["pc"], w=["cumpos"])
            S.op("dve", lambda ti=ti: V.tensor_tensor(out=accs[:], in0=accs[:], in1=PL[:, ti, :], op=ALU.add), r=["accs", "PL"], w=["accs"])
            S.op("dve", lambda ti=ti: V.tensor_tensor(out=rqt[:], in0=cumpos[:, ti, :], in1=CG[:, G, :], op=ALU.subtract),
                 r=["cumpos", "CG"], w=["rqt"])
            S.op("pe", lambda: PE_.transpose(out=pc[0:8, 128:256], in_=rqt[:], identity=C["ident"][:]), r=["rqt", "ident"], w=["pc"])
            S.op("act", lambda ti=ti: A.activation(out=rqst[:, ti * 128:(ti + 1) * 128], in_=pc[0:8, 128:256], func=AF.Copy, scale=-1.0),
                 r=["pc"], w=["rqst"])
        for kt in range(4 * G + 4):
            S.op("dve", lambda kt=kt: V.tensor_tensor(out=NB2[:, G, kt, :], in0=cumpos[:, kt, :], in1=CG[:, G, :], op=ALU.subtract),
                 r=["cumpos", "CG"], w=["NB2"])

    S.replay([S.capture(lambda: pn(0))])
    for g in range(NG):
        tm = S.capture(lambda: main(g))
        tn = S.capture(lambda: pn(g + 1)) if g + 1 < NG else []
        tc = S.capture(lambda: cum(g - 1)) if g >= 1 else []
        S.replay([tm, tn, tc])
    S.replay([S.capture(lambda: cum(NG - 1))])
    S.dma("sp", lambda: nc.sync.dma_start(out=R["NB"], in_=NB2[:].rearrange("p g t h -> p (g t h)")), r=["NB2"])
    S.dma("sp", lambda: nc.sync.dma_start(out=R["RQ"], in_=rqst[:]), r=["rqst"])
    S.flush()
    st.close()

def stage2(S, nc, T, P, C, R):
    st = ExitStack()
    sb, ps = mk(nc, st)
    V, A, PE_, PO = nc.vector, nc.scalar, nc.tensor, nc.gpsimd
    qaring = Ring("s2qa", [sb("s2_qa%d" % i, [80, SEQ], BF16) for i in range(2)])
    karing = Ring("s2ka", [sb("s2_ka%d" % i, [80, SEQ], BF16) for i in range(2)])
    vhring = Ring("s2vh", [sb("s2_vh%d" % i, [128, NT, 65], BF16) for i in range(2)])
    NB = sb("s2_nb", [128, 8, NT, 8])
    maskc = sb("s2_mask", [128, 4, 512], BF16)
    ptring = Ring("s2pt", [sb("s2_pt%d" % i, [128, 512], BF16) for i in range(4)])
    Oall = sb("s2_oall", [128, NT, D], BF16)
    osring = Ring("s2os", [sb("s2_os%d" % i, [65, 512]) for i in range(2)])
    rcring = Ring("s2rc", [sb("s2_rc%d" % i, [128, 4]) for i in range(2)])
    psring = Ring("s2ps", [ps("s2_ps%d" % i, [128, 512]) for i in range(4)])
    poring = Ring("s2po", [ps("s2_po%d" % i, [128, 512]) for i in range(2)])
    ptring2 = Ring("s2pq", [ps("s2_pq%d" % i, [128, 512]) for i in range(2)])

    S.dma("sp", lambda: nc.sync.dma_start(out=NB[:].rearrange("p g t h -> p (g t h)"), in_=R["NB"]), w=["NB"])
    S.dma("pool", lambda: PO.dma_start(out=maskc[:].rearrange("p a b -> p (a b)"), in_=T["maskc"]), w=["maskc"])
    for i in range(2):
        S.op("pool", lambda i=i: PO.memset(qaring.tens[i][64:80, :], 0.0), w=["s2qa%d" % i])

    def load_head(h):
        j, r0 = h // 2, (h % 2) * 64
        qa, kqa = qaring.next()
        ka, kka = karing.next()
        vh, kvh = vhring.next()
        S.dma("sp", lambda: nc.sync.dma_start(out=qa[0:64, :], in_=R["QT"][j, r0:r0 + 64, :]), w=[kqa])
        S.dma("sp", lambda: nc.sync.dma_start(out=ka[0:64, :], in_=R["KT"][j, r0:r0 + 64, :]), w=[kka])
        if h < 8:
            S.dma("sp", lambda: nc.sync.dma_start(out=qa[64:80, :], in_=R["NS"][:, h, :]), w=[kqa])
            S.dma("pool", lambda: PO.dma_start(out=ka[64:80, :], in_=T["BO"]), w=[kka])
        else:
            S.dma("sp", lambda: nc.sync.dma_start(out=qa[64:65, :], in_=R["RQ"][h - 8:h - 7, :]), w=[kqa])
            S.dma("pool", lambda: PO.dma_start(out=ka[64:80, :], in_=T["FO"]), w=[kka])
        for q4 in range(4):
            S.dma("sp", lambda q4=q4: nc.sync.dma_start(
                out=vh[:, q4 * 8:(q4 + 1) * 8, :],
                in_=R["V"][q4 * 8:(q4 + 1) * 8].rearrange("t p c -> p t c")[:, :, h * 65:(h + 1) * 65]), w=[kvh])
        return (qa, kqa, ka, kka, vh, kvh)

    units = []
    for h in range(16):
        for G in range(8):
            n = 4 * G + 4
            for kt in range(n):
                units.append((h, G, kt, n))
    heads = {0: load_head(0)}
    LAG = 3
    lagq = []
    pending = []

    def emit_pv(u):
        h, G, kt, n, pt, kpt, po, kpo, c0 = u
        qa, kqa, ka, kka, vh, kvh = heads[h]
        S.op("pe", lambda: PE_.matmul(po[0:65, c0:512], lhsT=vh[:, kt, :], rhs=pt[:, c0:512], start=(kt == 0), stop=(kt == n - 1)),
             r=[kvh, kpt], w=[kpo])
        if kt == n - 1:
            osb, kos = osring.next()
            rc, krc = rcring.next()
            pq, kpq = ptring2.next()
            S.op("dve", lambda: V.tensor_copy(out=osb[:], in_=po[0:65, :]), r=[kpo], w=[kos])

            def epi():
                for i in range(4):
                    S.op("pe", lambda i=i: PE_.transpose(out=pq[:, i * 65:(i + 1) * 65], in_=osb[0:65, i * 128:(i + 1) * 128],
                                                        identity=C["ident"][0:65, 0:65]), r=[kos, "ident"], w=[kpq])
                S.op("dve", lambda: V.reciprocal(out=rc[:], in_=pq[:, 0:260].rearrange("p (i c) -> p i c", c=65)[:, :, 64]),
                     r=[kpq], w=[krc])
                for i in range(4):
                    dst = Oall[:, 4 * G + i, h * 64:(h + 1) * 64]
                    if False:
                        S.op("act", lambda i=i, dst=dst: A.activation(out=dst, in_=pq[:, i * 65:i * 65 + 64], func=AF.Copy,
                                                                      scale=rc[:, i:i + 1]), r=[kpq, krc], w=["Oall"])
                    else:
                        S.op("dve", lambda i=i, dst=dst: V.tensor_scalar(out=dst, in0=pq[:, i * 65:i * 65 + 64], scalar1=rc[:, i:i + 1],
                                                                         scalar2=None, op0=ALU.mult), r=[kpq, krc], w=["Oall"])
            pending.append(epi)

    for idx, (h, G, kt, n) in enumerate(units):
        qa, kqa, ka, kka, vh, kvh = heads[h]
        pss, kps = psring.next()
        diag = kt >= 4 * G
        c0 = (kt - 4 * G) * 128 if diag else 0
        S.op("pe", lambda pss=pss, ka=ka, qa=qa, kt=kt, G=G, diag=diag, c0=c0: PE_.matmul(
            pss[:, c0:512], lhsT=ka[0:80, kt * 128:(kt + 1) * 128], rhs=qa[0:80, G * 512 + c0:(G + 1) * 512], start=True, stop=(not diag)),
            r=[kka, kqa], w=[kps])
        if diag:
            S.op("pe", lambda pss=pss, kt=kt, G=G, c0=c0: PE_.matmul(pss[:, c0:512], lhsT=C["identb"][:], rhs=maskc[:, kt - 4 * G, c0:512],
                                                                    start=False, stop=True), r=["identb", "maskc"], w=[kps])
        while pending:
            pending.pop(0)()
        pt, kpt = ptring.next()
        if h >= 8:
            S.op("act", lambda pt=pt, pss=pss, kt=kt, h=h, G=G, c0=c0: A.activation(out=pt[:, c0:512], in_=pss[:, c0:512], func=AF.Exp,
                                                                                    bias=NB[:, G, kt, h - 8:h - 7]), r=[kps, "NB"], w=[kpt])
        else:
            S.op("act", lambda pt=pt, pss=pss, c0=c0: A.activation(out=pt[:, c0:512], in_=pss[:, c0:512], func=AF.Exp), r=[kps], w=[kpt])
        if kt == 0:
            po, kpo = poring.next()
            cur_po = (po, kpo)
        lagq.append((h, G, kt, n, pt, kpt, cur_po[0], cur_po[1], c0))
        if len(lagq) > LAG:
            emit_pv(lagq.pop(0))
        if G == 0 and kt == LAG and h + 1 < 16:
            heads[h + 1] = load_head(h + 1)
    while lagq:
        emit_pv(lagq.pop(0))
    while pending:
        pending.pop(0)()
    Od = R["O"].rearrange("(t p) c -> p t c", p=128)
    for q4 in range(4):
        S.dma("sp", lambda q4=q4: nc.sync.dma_start(out=Od[:, q4 * 8:(q4 + 1) * 8, :], in_=Oall[:, q4 * 8:(q4 + 1) * 8, :]), r=["Oall"])
    S.flush()
    st.close()

def stage3a(S, nc, T, P, C, R):
    st = ExitStack()
    sb, ps = mk(nc, st)
    V, A, PE_, PO = nc.vector, nc.scalar, nc.tensor, nc.gpsimd
    wo = sb("s3_wo", [128, 8, D], BF16)
    oring = Ring("s3o", [sb("s3_o%d" % i, [128, D], BF16) for i in range(4)])
    otring = Ring("s3ot", [sb("s3_ot%d" % i, [128, 8, 128], BF16) for i in range(2)])
    xring = Ring("s3x", [sb("s3_x%d" % i, [128, D]) for i in range(4)])
    xoring = Ring("s3xo", [sb("s3_xo%d" % i, [128, D]) for i in range(4)])
    Wp = postnorm_work(nc, st, "s3p_")
    ptr = Ring("s3ptr", [ps("s3_ptr%d" % i, [128, 1024], BF16) for i in range(2)])
    pyr = Ring("s3py", [ps("s3_py%d" % i, [128, 512]) for i in range(4)])
    for i in range(2):
        S.dma("pool", lambda i=i: PO.dma_start(out=wo[:, :, i * 512:(i + 1) * 512],
                                               in_=T["attn_w_out"][:, i * 512:(i + 1) * 512].rearrange("(kc p) n -> p kc n", p=128)),
              w=["wo"])
    def tile(ti):
        ot_, ko = oring.next()
        S.dma("sp", lambda ot_=ot_, ti=ti: nc.sync.dma_start(out=ot_[:], in_=R["O"][ti * 128:(ti + 1) * 128, :]), w=[ko])
        xt, kx = xring.next()
        S.dma("sp", lambda xt=xt, ti=ti: nc.sync.dma_start(out=xt[:], in_=T["x"][ti * 128:(ti + 1) * 128, :]), w=[kx])
        pt, kpt = ptr.next()
        for kc in range(8):
            S.op("pe", lambda pt=pt, ot_=ot_, kc=kc: PE_.transpose(out=pt[:, kc * 128:(kc + 1) * 128], in_=ot_[:, kc * 128:(kc + 1) * 128],
                                                                  identity=C["identb"][:]), r=[ko, "identb"], w=[kpt])
        oT, koT = otring.next()
        S.op("dve", lambda oT=oT, pt=pt: V.tensor_copy(out=oT[:, 0:4, :], in_=pt[:, 0:512].rearrange("p (k q) -> p k q", q=128)),
             r=[kpt], w=[koT + "a"])
        S.op("dve", lambda oT=oT, pt=pt: V.tensor_copy(out=oT[:, 4:8, :], in_=pt[:, 512:1024].rearrange("p (k q) -> p k q", q=128)),
             r=[kpt], w=[koT + "b"])
        pys = []
        for hf in range(2):
            py, kpy = pyr.next()
            pys.append((py, kpy))
            for kc in range(8):
                S.op("pe", lambda py=py, oT=oT, kc=kc, hf=hf: PE_.matmul(py[:], lhsT=oT[:, kc, :], rhs=wo[:, kc, hf * 512:(hf + 1) * 512],
                                                                      start=(kc == 0), stop=(kc == 7)), r=[koT + "a", koT + "b", "wo"], w=[kpy])
        xo, kxo = xoring.next()
        postnorm_tile(S, nc, Wp, lambda hf, pys=pys: pys[hf][0][:], [pys[0][1], pys[1][1]], xt, kx, xo, kxo, 0, P)
        S.dma("pool", lambda xo=xo, ti=ti: PO.dma_start(out=R["x1a"][ti * 128:(ti + 1) * 128, :], in_=xo[:]), r=[kxo])

    for ti in range(0, NT, 2):
        ta = S.capture(lambda: tile(ti))
        tb = S.capture(lambda: tile(ti + 1))
        S.replay([ta, tb])
    S.flush()
    st.close()


def ffn_pass(S, nc, T, P, C, pfx, x_src, x_dst, sub, experts, F, router=None):
    V, A, PE_, PO = nc.vector, nc.scalar, nc.tensor, nc.gpsimd
    NTL = 16
    st0 = ExitStack()
    sb0, _ = mk(nc, st0)
    hT = sb0(pfx + "hT", [128, 8, NTL * 128], BF16)
    acc = sb0(pfx + "acc", [128, NTL, D])
    G = sb0(pfx + "G", [128, NTL, 8]) if router is not None else None
    st = ExitStack()
    sb, ps = mk(nc, st)
    NW = 4 if router is None else 2
    xring = Ring(pfx + "x", [sb(pfx + "x%d" % i, [128, D]) for i in range(2 * NW)])
    Wn = prenorm_work(nc, st, pfx + "n_", ntp=NW, nset=NW)
    if router is not None:
        RT = [dict(h32=sb(pfx + "h32_%d" % i, [128, 8, 128]), lg=sb(pfx + "lg%d" % i, [128, 8]), t8=sb(pfx + "t8%d" % i, [128, 8]),
                   nmx=sb(pfx + "nmx%d" % i, [128, 1]), msk=sb(pfx + "msk%d" % i, [128, 8]), ex=sb(pfx + "ex%d" % i, [128, 8]),
                   den=sb(pfx + "den%d" % i, [128, 2]), pl=ps(pfx + "pl%d" % i, [128, 512])) for i in range(2)]
        wr = sb(pfx + "wr", [128, 8, 8])
        br = sb(pfx + "br", [128, 8])
        S.dma("sp", lambda: nc.sync.dma_start(out=wr[:], in_=router[0].rearrange("(kc p) e -> p kc e", p=128)), w=["wr"])
        S.dma("sp", lambda: nc.sync.dma_start(out=br[:], in_=router[1].partition_broadcast(128)), w=["br"])

    def tile_a(t):
        xt, kx = xring.next()
        S.dma("sp", lambda: nc.sync.dma_start(out=xt[:], in_=x_src[t * 128:(t + 1) * 128, :]), w=[kx])
        kd = pfx + "hTt%d" % t
        if router is None:
            prenorm_tile(S, nc, Wn, xt[:], kx, sub, P, C, lambda kc: hT[:, kc, t * 128:(t + 1) * 128], kd)
            return
        q = t % 2
        rt = RT[q]
        h32, lg, t8, nmx, msk, ex, den, pl = (rt[k] for k in ("h32", "lg", "t8", "nmx", "msk", "ex", "den", "pl"))
        sfx = "_r%d" % q
        prenorm_tile(S, nc, Wn, xt[:], kx, sub, P, C, lambda kc: hT[:, kc, t * 128:(t + 1) * 128], kd,
                     want32=lambda kc: h32[:, kc, :], k32=pfx + "h32" + sfx)
        for kc in range(8):
            S.op("pe", lambda kc=kc: PE_.matmul(pl[:, 0:8], lhsT=h32[:, kc, :], rhs=wr[:, kc, :], start=(kc == 0), stop=(kc == 7)),
                 r=[pfx + "h32" + sfx + "_%d" % kc, "wr"], w=["pl" + sfx])
        S.op("dve", lambda: V.tensor_tensor(out=lg[:], in0=pl[:, 0:8], in1=br[:], op=ALU.add), r=["pl" + sfx, "br"], w=["lg" + sfx])
        S.op("dve", lambda: V.max(out=t8[:], in_=lg[:]), r=["lg" + sfx], w=["t8" + sfx])
        S.op("dve", lambda: V.tensor_scalar(out=nmx[:], in0=t8[:, 0:1], scalar1=-1.0, scalar2=None, op0=ALU.mult), r=["t8" + sfx], w=["nmx" + sfx])
        S.op("dve", lambda: V.tensor_scalar(out=msk[:], in0=lg[:], scalar1=t8[:, 1:2], scalar2=None, op0=ALU.is_ge),
             r=["lg" + sfx, "t8" + sfx], w=["msk" + sfx])
        S.op("act", lambda: A.activation(out=ex[:], in_=lg[:], func=AF.Exp, bias=nmx[:, 0:1]), r=["lg" + sfx, "nmx" + sfx], w=["ex" + sfx])
        S.op("dve", lambda: V.tensor_tensor(out=ex[:], in0=ex[:], in1=msk[:], op=ALU.mult), r=["ex" + sfx, "msk" + sfx], w=["ex" + sfx])
        S.op("dve", lambda: V.tensor_reduce(out=den[:, 0:1], in_=ex[:], axis=AX.X, op=ALU.add), r=["ex" + sfx], w=["den" + sfx])
        S.op("dve", lambda: V.reciprocal(out=den[:, 1:2], in_=den[:, 0:1]), r=["den" + sfx], w=["den2" + sfx])
        S.op("dve", lambda: V.tensor_scalar(out=G[:, t, :], in0=ex[:], scalar1=den[:, 1:2], scalar2=None, op0=ALU.mult),
             r=["ex" + sfx, "den2" + sfx], w=["G%d" % t])

    NI = 4 if router is None else 2
    for t in range(0, NTL, NI):
        S.replay([S.capture(lambda i=i: tile_a(t + i)) for i in range(NI)])
    S.flush()
    st.close()
    st = ExitStack()
    sb, ps = mk(nc, st)
    wgr = Ring(pfx + "wg", [sb(pfx + "wg%d" % i, [128, 8, 512], BF16) for i in range(2)])
    wur = Ring(pfx + "wu", [sb(pfx + "wu%d" % i, [128, 8, 512], BF16) for i in range(2)])
    wdr = Ring(pfx + "wd", [sb(pfx + "wd%d" % i, [128, 4, D], BF16) for i in range(2)])
    h1 = sb(pfx + "h1", [128, 4, NTL * 128], BF16)
    sgr = Ring(pfx + "sg", [sb(pfx + "sg%d" % i, [128, 512]) for i in range(2)])
    pgr = Ring(pfx + "pg", [ps(pfx + "pg%d" % i, [128, 512]) for i in range(2)])
    pur = Ring(pfx + "pu", [ps(pfx + "pu%d" % i, [128, 512]) for i in range(2)])
    pdr = Ring(pfx + "pd", [ps(pfx + "pd%d" % i, [128, 512]) for i in range(4)])
    first = True
    for e, (Wg, Wu, Wd) in enumerate(experts):
        nfg = (F + 511) // 512
        for fg in range(nfg):
            f0 = fg * 512
            nch = min(4, (F - f0) // 128)
            wg, kwg = wgr.next()
            wu, kwu = wur.next()
            wd, kwd = wdr.next()
            S.dma("pool", lambda wg=wg, Wg=Wg, f0=f0, nch=nch: PO.dma_start(
                out=wg[:, :, 0:nch * 128], in_=Wg[:, f0:f0 + nch * 128].rearrange("(kc p) n -> p kc n", p=128)), w=[kwg])
            S.dma("pool", lambda wu=wu, Wu=Wu, f0=f0, nch=nch: PO.dma_start(
                out=wu[:, :, 0:nch * 128], in_=Wu[:, f0:f0 + nch * 128].rearrange("(kc p) n -> p kc n", p=128)), w=[kwu])
            S.dma("pool", lambda wd=wd, Wd=Wd, f0=f0, nch=nch: PO.dma_start(
                out=wd[:, 0:nch, :], in_=Wd[f0:f0 + nch * 128, :].rearrange("(c p) n -> p c n", p=128)), w=[kwd])
            for c in range(nch):
                for tg in range(4):
                    pg_, kpg = pgr.next()
                    pu_, kpu = pur.next()
                    for kc in range(8):
                        S.op("pe", lambda pg_=pg_, wg=wg, kc=kc, c=c, tg=tg: PE_.matmul(
                            pg_[:], lhsT=wg[:, kc, c * 128:(c + 1) * 128], rhs=hT[:, kc, tg * 512:(tg + 1) * 512],
                            start=(kc == 0), stop=(kc == 7)), r=[kwg], w=[kpg])
                    for kc in range(8):
                        S.op("pe", lambda pu_=pu_, wu=wu, kc=kc, c=c, tg=tg: PE_.matmul(
                            pu_[:], lhsT=wu[:, kc, c * 128:(c + 1) * 128], rhs=hT[:, kc, tg * 512:(tg + 1) * 512],
                            start=(kc == 0), stop=(kc == 7)), r=[kwu], w=[kpu])
                    sg, ksg = sgr.next()
                    S.op("act", lambda sg=sg, pg_=pg_: A.activation(out=sg[:], in_=pg_[:], func=AF.Silu), r=[kpg], w=[ksg])
                    S.op("dve", lambda sg=sg, pu_=pu_, c=c, tg=tg: V.tensor_tensor(
                        out=h1[:, c, tg * 512:(tg + 1) * 512], in0=pu_[:], in1=sg[:], op=ALU.mult), r=[kpu, ksg],
                        w=[pfx + "h1_%d_%d" % (c, tg)])
            for t in range(NTL):
                for hf in range(2):
                    pd, kpd = pdr.next()
                    for c in range(nch):
                        S.op("pe", lambda pd=pd, wd=wd, c=c, t=t, hf=hf, nch=nch: PE_.matmul(
                            pd[:], lhsT=h1[:, c, t * 128:(t + 1) * 128], rhs=wd[:, c, hf * 512:(hf + 1) * 512],
                            start=(c == 0), stop=(c == nch - 1)), r=[kwd, pfx + "h1_%d_%d" % (c, t // 4)], w=[kpd])
                    dst = acc[:, t, hf * 512:(hf + 1) * 512]
                    ka = pfx + "acc_%d_%d" % (t, hf)
                    if first:
                        if G is None:
                            S.op("dve", lambda pd=pd, dst=dst: V.tensor_copy(out=dst, in_=pd[:]), r=[kpd], w=[ka])
                        else:
                            S.op("dve", lambda pd=pd, dst=dst, t=t, e=e: V.tensor_scalar(
                                out=dst, in0=pd[:], scalar1=G[:, t, e:e + 1], scalar2=None, op0=ALU.mult), r=[kpd], w=[ka])
                    else:
                        if G is None:
                            S.op("dve", lambda pd=pd, dst=dst: V.tensor_tensor(out=dst, in0=pd[:], in1=dst, op=ALU.add), r=[kpd, ka], w=[ka])
                        else:
                            S.op("dve", lambda pd=pd, dst=dst, t=t, e=e: V.scalar_tensor_tensor(
                                out=dst, in0=pd[:], scalar=G[:, t, e:e + 1], in1=dst, op0=ALU.mult, op1=ALU.add), r=[kpd, ka], w=[ka])
            first = False
    S.flush()
    st.close()
    st = ExitStack()
    sb, ps = mk(nc, st)
    xring = Ring(pfx + "cx", [sb(pfx + "cx%d" % i, [128, D]) for i in range(8)])
    xoring = Ring(pfx + "cxo", [sb(pfx + "cxo%d" % i, [128, D]) for i in range(8)])
    Wp = postnorm_work(nc, st, pfx + "p_", nset=4)
    def tile_c(t):
        xt, kx = xring.next()
        S.dma("sp", lambda xt=xt, t=t: nc.sync.dma_start(out=xt[:], in_=x_src[t * 128:(t + 1) * 128, :]), w=[kx])
        xo, kxo = xoring.next()
        postnorm_tile(S, nc, Wp, lambda hf, t=t: acc[:, t, hf * 512:(hf + 1) * 512], [], xt, kx, xo, kxo, sub, P)
        S.dma("pool", lambda xo=xo, t=t: PO.dma_start(out=x_dst[t * 128:(t + 1) * 128, :], in_=xo[:]), r=[kxo])

    for t in range(0, NTL, 4):
        S.replay([S.capture(lambda i=i: tile_c(t + i)) for i in range(4)])
    S.flush()
    st.close()
    st0.close()

def stage4(S, nc, T, P, C, R):
    st = ExitStack()
    sb, ps = mk(nc, st)
    V, A, PE_, PO = nc.vector, nc.scalar, nc.tensor, nc.gpsimd
    DR = 1280
    wu = sb("s4_wu", [128, 8, DR], BF16)
    wa = sb("s4_wa", [128, 10, 128], BF16)
    wx = sb("s4_wx", [128, 10, 128], BF16)
    prow = sb("s4_prow", [1, 8 * DR])
    one11 = sb("s4_one", [1, 1])
    par = sb("s4_par", [128, 80])
    ex = sb("s4_ex", [128, 10])
    tq = sb("s4_tq", [128, 10])
    nsp8 = sb("s4_nsp8", [128, 10])
    nsp16 = sb("s4_nsp16", [128, 10])
    hcar = sb("s4_hcar", [128, 10])
    hba = sb("s4_hba", [128, 10])
    hbx = sb("s4_hbx", [128, 10])
    h8 = sb("s4_h8", [128, 10])
    h16 = sb("s4_h16", [128, 10])
    xring = Ring("s4x", [sb("s4_x%d" % i, [128, D]) for i in range(2)])
    hring = Ring("s4h", [sb("s4_h%d" % i, [128, 8, 512], BF16) for i in range(2)])
    Wn = prenorm_work(nc, st, "s4n_")
    ub = sb("s4_ub", [128, 10, 515])
    cv_r = Ring("s4cv", [sb("s4_cv%d" % i, [128, 512]) for i in range(4)])
    cvb_r = Ring("s4cvb", [sb("s4_cvb%d" % i, [128, 512], BF16) for i in range(2)])
    rg_r = Ring("s4rg", [sb("s4_rg%d" % i, [128, 512]) for i in range(4)])
    ig_r = Ring("s4ig", [sb("s4_ig%d" % i, [128, 512]) for i in range(4)])
    av_r = Ring("s4av", [sb("s4_av%d" % i, [128, 512]) for i in range(2)])
    a2_r = Ring("s4a2", [sb("s4_a2%d" % i, [128, 512]) for i in range(2)])
    xin_r = Ring("s4xin", [sb("s4_xin%d" % i, [128, 512]) for i in range(2)])
    hsr = Ring("s4hs", [sb("s4_hs%d" % i, [128, 512]) for i in range(2)])
    pur = Ring("s4pu", [ps("s4_pu%d" % i, [128, 512]) for i in range(2)])
    pr_r = Ring("s4pr", [ps("s4_pr%d" % i, [128, 512]) for i in range(2)])
    px_r = Ring("s4px", [ps("s4_px%d" % i, [128, 512]) for i in range(2)])
    pp = pr_r.tens[0]

    w_in = T["lru_w_in"]
    for i in range(2):
        S.dma("pool", lambda i=i: PO.dma_start(out=wu[:, :, i * 640:(i + 1) * 640],
                                               in_=w_in[:, DR + i * 640:DR + (i + 1) * 640].rearrange("(kc p) n -> p kc n", p=128)), w=["wu"])
    S.dma("pool", lambda: PO.dma_start(out=wa[:], in_=T["lru_w_a"].rearrange("n c d -> c n d")), w=["wa"])
    S.dma("pool", lambda: PO.dma_start(out=wx[:], in_=T["lru_w_x"].rearrange("n c d -> c n d")), w=["wx"])
    S.dma("sp", lambda: nc.sync.dma_start(out=prow[0:1, 0:4 * DR], in_=T["lru_conv_w"]), w=["prow"])
    for r, nm in enumerate(["lru_conv_b", "lru_b_a", "lru_b_x", "lru_lambda"]):
        S.dma("sp", lambda r=r, nm=nm: nc.sync.dma_start(out=prow[0:1, (4 + r) * DR:(5 + r) * DR], in_=T[nm]), w=["prow"])
    S.op("pool", lambda: PO.memset(one11[:], 1.0), w=["one11"])
    S.op("pool", lambda: PO.memset(ub[:], 0.0), w=["ub"])
    S.op("pool", lambda: PO.memset(hcar[:], 0.0), w=["hcar%d" % c for c in range(10)])
    for r in range(8):
        for c in range(10):
            S.op("pe", lambda r=r, c=c: PE_.matmul(pp[:, r * 10 + c:r * 10 + c + 1], lhsT=prow[0:1, r * DR + c * 128:r * DR + (c + 1) * 128],
                                                  rhs=one11[0:1, 0:1], start=True, stop=True), r=["prow", "one11"], w=["s4pr0"])
    S.op("dve", lambda: V.tensor_copy(out=par[:], in_=pp[:, 0:80]), r=["s4pr0"], w=["par"])
    S.op("act", lambda: A.activation(out=ex[:], in_=par[:, 70:80], func=AF.Exp, scale=-1.0), r=["par"], w=["ex"])
    S.op("dve", lambda: V.tensor_scalar(out=tq[:], in0=ex[:], scalar1=-1.0 / 3.0, scalar2=0.5, op0=ALU.mult, op1=ALU.add), r=["ex"], w=["tq"])
    S.op("dve", lambda: V.tensor_tensor(out=tq[:], in0=tq[:], in1=ex[:], op=ALU.mult), r=["tq", "ex"], w=["tq"])
    S.op("dve", lambda: V.tensor_scalar(out=tq[:], in0=tq[:], scalar1=-1.0, scalar2=1.0, op0=ALU.mult, op1=ALU.add), r=["tq"], w=["tq"])
    S.op("dve", lambda: V.tensor_tensor(out=tq[:], in0=tq[:], in1=ex[:], op=ALU.mult), r=["tq", "ex"], w=["tq"])
    S.op("dve", lambda: V.tensor_scalar(out=nsp8[:], in0=tq[:], scalar1=-8.0, scalar2=None, op0=ALU.mult), r=["tq"], w=["nsp8"])
    S.op("dve", lambda: V.tensor_scalar(out=nsp16[:], in0=tq[:], scalar1=-16.0, scalar2=None, op0=ALU.mult), r=["tq"], w=["nsp16"])
    S.op("dve", lambda: V.tensor_scalar(out=h8[:], in0=tq[:], scalar1=-4.0, scalar2=None, op0=ALU.mult), r=["tq"], w=["h8"])
    S.op("dve", lambda: V.tensor_scalar(out=h16[:], in0=tq[:], scalar1=-8.0, scalar2=None, op0=ALU.mult), r=["tq"], w=["h16"])
    S.op("dve", lambda: V.tensor_scalar(out=hba[:], in0=par[:, 50:60], scalar1=0.5, scalar2=None, op0=ALU.mult), r=["par"], w=["hba"])
    S.op("dve", lambda: V.tensor_scalar(out=hbx[:], in0=par[:, 60:70], scalar1=0.5, scalar2=None, op0=ALU.mult), r=["par"], w=["hbx"])
    x1 = R["x1"]
    ctx = {}

    def pn(g):
        hT, kh = hring.next()
        hkeys = []
        for t in range(4):
            ti = 4 * g + t
            xt, kx = xring.next()
            S.dma("sp", lambda xt=xt, ti=ti: nc.sync.dma_start(out=xt[:], in_=x1[ti * 128:(ti + 1) * 128, :]), w=[kx])
            kd = "%st%d" % (kh, t)
            prenorm_tile(S, nc, Wn, xt[:], kx, 2, P, C, lambda kc, hT=hT, t=t: hT[:, kc, t * 128:(t + 1) * 128], kd)
            hkeys += k8(kd)
        ctx[g] = (hT, hkeys)

    def main(g):
        hT, hkeys = ctx[g]

        def stage_a(c):
            pu, kpu = pur.next()
            for kc in range(8):
                S.op("pe", lambda kc=kc: PE_.matmul(pu[:], lhsT=wu[:, kc, c * 128:(c + 1) * 128], rhs=hT[:, kc, :],
                                                    start=(kc == 0), stop=(kc == 7)), r=["wu"] + hkeys, w=[kpu])
            kub = "ub%d" % c
            cv, kcv = cv_r.next()
            cvb, kcvb = cvb_r.next()
            pr, kpr = pr_r.next()
            px, kpx = px_r.next()
            rg, krg = rg_r.next()
            ig, kig = ig_r.next()
            S.op("act", lambda: A.activation(out=ub[:, c, 3:515], in_=pu[:], func=AF.Copy), r=[kpu], w=[kub])
            S.op("dve", lambda: V.tensor_scalar(out=cv[:], in0=ub[:, c, 3:515], scalar1=par[:, 30 + c:31 + c], scalar2=par[:, 40 + c:41 + c],
                                                op0=ALU.mult, op1=ALU.add), r=[kub, "par"], w=[kcv])
            for j in range(3):
                S.op("dve", lambda j=j: V.scalar_tensor_tensor(out=cv[:], in0=ub[:, c, j:j + 512], scalar=par[:, j * 10 + c:j * 10 + c + 1],
                                                               in1=cv[:], op0=ALU.mult, op1=ALU.add), r=[kub, "par", kcv], w=[kcv])
            S.op("pool", lambda: PO.tensor_copy(out=ub[:, c, 0:3], in_=ub[:, c, 512:515]), r=[kub], w=[kub])
            S.op("pool", lambda: PO.tensor_copy(out=cvb[:], in_=cv[:]), r=[kcv], w=[kcvb])
            S.op("pe", lambda: PE_.matmul(pr[:], lhsT=wa[:, c, :], rhs=cvb[:], start=True, stop=True), r=["wa", kcvb], w=[kpr])
            S.op("pe", lambda: PE_.matmul(px[:], lhsT=wx[:, c, :], rhs=cvb[:], start=True, stop=True), r=["wx", kcvb], w=[kpx])
            S.op("act", lambda: A.activation(out=rg[:], in_=pr[:], func=AF.Tanh, scale=0.5, bias=hba[:, c:c + 1]), r=[kpr, "hba"], w=[krg])
            S.op("act", lambda: A.activation(out=ig[:], in_=px[:], func=AF.Tanh, scale=0.5, bias=hbx[:, c:c + 1]), r=[kpx, "hbx"], w=[kig])
            return (c, cv, kcv, rg, krg, ig, kig)

        def stage_b(stt):
            c, cv, kcv, rg, krg, ig, kig = stt
            av, kav = av_r.next()
            a2, ka2 = a2_r.next()
            xin, kxin = xin_r.next()
            S.op("act", lambda: A.activation(out=av[:], in_=rg[:], func=AF.Exp, scale=h8[:, c:c + 1], bias=h8[:, c:c + 1]),
                 r=[krg, "h8"], w=[kav])
            S.op("act", lambda: A.activation(out=a2[:], in_=rg[:], func=AF.Exp, scale=h16[:, c:c + 1], bias=h16[:, c:c + 1]),
                 r=[krg, "h16"], w=[ka2])
            S.op("act", lambda: A.activation(out=a2[:], in_=a2[:], func=AF.Sqrt, scale=-0.25, bias=0.25), r=[ka2], w=[ka2])
            S.op("dve", lambda: V.scalar_tensor_tensor(out=xin[:], in0=ig[:], scalar=1.0, in1=cv[:], op0=ALU.add, op1=ALU.mult),
                 r=[kig, kcv], w=[kxin])
            S.op("dve", lambda: V.tensor_tensor(out=xin[:], in0=xin[:], in1=a2[:], op=ALU.mult), r=[kxin, ka2], w=[kxin])
            hs_, khs = hsr.next()
            khc = "hcar%d" % c
            S.op("dve", lambda: V.tensor_tensor_scan(out=hs_[:], data0=av[:], data1=xin[:], initial=hcar[:, c:c + 1],
                                                     op0=ALU.mult, op1=ALU.add), r=[kav, kxin, khc], w=[khs])
            S.op("dve", lambda: V.tensor_copy(out=hcar[:, c:c + 1], in_=hs_[:, 511:512]), r=[khs], w=[khc])
            S.dma("sp", lambda: nc.sync.dma_start(out=R["hs"][c, :, g * 512:(g + 1) * 512], in_=hs_[:]), r=[khs])

        sts = {}

        def run_a(c):
            sts[c] = stage_a(c)

        S.replay([S.capture(lambda: run_a(0)), S.capture(lambda: run_a(1))])
        for c in range(0, 10, 2):
            thr = [S.capture(lambda: stage_b(sts[c])), S.capture(lambda: stage_b(sts[c + 1]))]
            if c + 2 < 10:
                thr += [S.capture(lambda: run_a(c + 2)), S.capture(lambda: run_a(c + 3))]
            S.replay(thr)

    S.replay([S.capture(lambda: pn(0))])
    for g in range(8):
        tm = S.capture(lambda: main(g))
        tn = S.capture(lambda: pn(g + 1)) if g + 1 < 8 else []
        S.replay([tm, tn])
    S.flush()
    st.close()


def stage5(S, nc, T, P, C, R):
    st = ExitStack()
    sb, ps = mk(nc, st)
    V, A, PE_, PO = nc.vector, nc.scalar, nc.tensor, nc.gpsimd
    DR = 1280
    wg = sb("s5_wg", [128, 8, DR], BF16)
    wo = sb("s5_wo", [128, 10, D], BF16)
    hsel = sb("s5_hsel", [128, 2])
    xar = Ring("s5xa", [sb("s5_xa%d" % i, [128, D]) for i in range(2)])
    xbr = Ring("s5xb", [sb("s5_xb%d" % i, [128, D]) for i in range(2)])
    xownr = Ring("s5xown", [sb("s5_xown%d" % i, [128, 4, D]) for i in range(2)])
    hring = Ring("s5h", [sb("s5_h%d" % i, [128, 8, 512], BF16) for i in range(2)])
    Wn = prenorm_work(nc, st, "s5n_")
    Wp = postnorm_work(nc, st, "s5p_")
    x2r = Ring("s5x2", [sb("s5_x2s%d" % i, [128, 512]) for i in range(2)])
    tqr = Ring("s5tq", [sb("s5_tq%d" % i, [128, 512]) for i in range(2)])
    sgr = Ring("s5sg", [sb("s5_sg%d" % i, [128, 512]) for i in range(2)])
    hsa = Ring("s5hsa", [sb("s5_hsa%d" % i, [128, 512]) for i in range(2)])
    hsb = Ring("s5hsb", [sb("s5_hsb%d" % i, [128, 512]) for i in range(2)])
    yT = sb("s5_yT", [128, 10, 512], BF16)
    xoring = Ring("s5xo", [sb("s5_xo%d" % i, [128, D]) for i in range(2)])
    pgr = Ring("s5pg", [ps("s5_pg%d" % i, [128, 512]) for i in range(2)])
    pyr = Ring("s5py", [ps("s5_py%d" % i, [128, 512]) for i in range(4)])

    w_in = T["lru_w_in"]
    for i in range(2):
        S.dma("pool", lambda i=i: PO.dma_start(out=wg[:, :, i * 640:(i + 1) * 640],
                                               in_=w_in[:, i * 640:(i + 1) * 640].rearrange("(kc p) n -> p kc n", p=128)), w=["wg"])
        S.dma("pool", lambda i=i: PO.dma_start(out=wo[:, :, i * 512:(i + 1) * 512],
                                               in_=T["lru_w_out"][:, i * 512:(i + 1) * 512].rearrange("(c p) n -> p c n", p=128)), w=["wo"])
    S.dma("sp", lambda: nc.sync.dma_start(out=hsel[:], in_=T["hsel"]), w=["hsel"])
    x1 = R["x1"]
    ctx = {}

    def pn(g):
        hT, kh = hring.next()
        xown, kxw = xownr.next()
        hkeys = []
        for t in range(4):
            ti = 4 * g + t
            xa, kxa = xar.next()
            xb, kxb = xbr.next()
            S.dma("sp", lambda xa=xa, ti=ti: nc.sync.dma_start(out=xa[:], in_=x1[ti * 128:(ti + 1) * 128, :]), w=[kxa])
            S.dma("sp", lambda xb=xb, ti=ti: nc.sync.dma_start(out=xb[:], in_=x1[2048 + ti * 128:2048 + (ti + 1) * 128, :]), w=[kxb])
            kxo = "%s_%d" % (kxw, t)
            S.op("dve", lambda xa=xa, t=t: V.tensor_scalar(out=xown[:, t, :], in0=xa[:], scalar1=hsel[:, 1:2], scalar2=None, op0=ALU.mult),
                 r=[kxa, "hsel"], w=[kxo])
            S.op("dve", lambda xb=xb, t=t: V.scalar_tensor_tensor(out=xown[:, t, :], in0=xb[:], scalar=hsel[:, 0:1], in1=xown[:, t, :],
                                                                  op0=ALU.mult, op1=ALU.add), r=[kxb, "hsel", kxo], w=[kxo])
            kd = "%st%d" % (kh, t)
            prenorm_tile(S, nc, Wn, xown[:, t, :], kxo, 2, P, C, lambda kc, t=t: hT[:, kc, t * 128:(t + 1) * 128], kd)
            hkeys += k8(kd)
        ctx[g] = (hT, hkeys, xown, kxw)

    def chunk(g, c):
        hT, hkeys, xown, kxw = ctx[g]
        pg_, kpg = pgr.next()
        for kc in range(8):
            S.op("pe", lambda kc=kc: PE_.matmul(pg_[:], lhsT=wg[:, kc, c * 128:(c + 1) * 128], rhs=hT[:, kc, :],
                                                start=(kc == 0), stop=(kc == 7)), r=["wg"] + hkeys, w=[kpg])
        ha, kha = hsa.next()
        hb, khb = hsb.next()
        x2s, kx2 = x2r.next()
        tq, ktq = tqr.next()
        sg, ksg = sgr.next()
        S.dma("sp", lambda: nc.sync.dma_start(out=ha[:], in_=R["hs"][c, :, g * 512:(g + 1) * 512]), w=[kha])
        S.dma("sp", lambda: nc.sync.dma_start(out=hb[:], in_=R["hs"][c, :, 2048 + g * 512:2048 + (g + 1) * 512]), w=[khb])
        S.op("act", lambda: A.activation(out=x2s[:], in_=pg_[:], func=AF.Square), r=[kpg], w=[kx2])
        S.op("dve", lambda: V.tensor_scalar(out=tq[:], in0=x2s[:], scalar1=0.044715, scalar2=1.0, op0=ALU.mult, op1=ALU.add),
             r=[kx2], w=[ktq])
        S.op("dve", lambda: V.tensor_tensor(out=tq[:], in0=pg_[:], in1=tq[:], op=ALU.mult), r=[kpg, ktq], w=[ktq])
        S.op("act", lambda: A.activation(out=sg[:], in_=tq[:], func=AF.Sigmoid, scale=1.5957691216057308), r=[ktq], w=[ksg])
        S.op("dve", lambda: V.tensor_tensor(out=sg[:], in0=pg_[:], in1=sg[:], op=ALU.mult), r=[kpg, ksg], w=[ksg])
        S.op("act", lambda: A.activation(out=ha[:], in_=ha[:], func=AF.Copy, scale=hsel[:, 1:2]), r=[kha, "hsel"], w=[kha])
        S.op("dve", lambda: V.scalar_tensor_tensor(out=ha[:], in0=hb[:], scalar=hsel[:, 0:1], in1=ha[:],
                                                   op0=ALU.mult, op1=ALU.add), r=[khb, "hsel", kha], w=[kha])
        S.op("dve", lambda: V.tensor_tensor(out=yT[:, c, :], in0=sg[:], in1=ha[:], op=ALU.mult), r=[ksg, kha], w=["yT%d" % c])

    ykeys = ["yT%d" % c for c in range(10)]

    def outtile(g, t):
        hT, hkeys, xown, kxw = ctx[g]
        ti = 4 * g + t
        pys = []
        for hf in range(2):
            py, kpy = pyr.next()
            pys.append((py, kpy))
            for c in range(10):
                S.op("pe", lambda py=py, c=c, hf=hf: PE_.matmul(py[:], lhsT=yT[:, c, t * 128:(t + 1) * 128],
                                                              rhs=wo[:, c, hf * 512:(hf + 1) * 512], start=(c == 0), stop=(c == 9)),
                     r=ykeys + ["wo"], w=[kpy])
        xo, kxo2 = xoring.next()
        postnorm_tile(S, nc, Wp, lambda hf: pys[hf][0][:], [pys[0][1], pys[1][1]], xown[:, t, :], "%s_%d" % (kxw, t), xo, kxo2, 2, P)
        S.dma("pool", lambda: PO.dma_start(out=R["x2"][ti * 128:(ti + 1) * 128, :], in_=xo[:]), r=[kxo2])

    S.replay([S.capture(lambda: pn(0))])
    for g in range(4):
        tn = S.capture(lambda: pn(g + 1)) if g + 1 < 4 else []
        nsl = 7
        sl = [tn[i * len(tn) // nsl:(i + 1) * len(tn) // nsl] for i in range(nsl)]
        for p in range(5):
            S.replay([S.capture(lambda: chunk(g, 2 * p)), S.capture(lambda: chunk(g, 2 * p + 1)), sl[p]])
        for p in range(2):
            S.replay([S.capture(lambda: outtile(g, 2 * p)), S.capture(lambda: outtile(g, 2 * p + 1)), sl[5 + p]])
    S.flush()
    st.close()


def host_consts(half):
    bf = ml_dtypes.bfloat16
    c = {}
    c["ident"] = np.eye(128, dtype=np.float32)
    c["identb"] = np.eye(128, dtype=np.float32).astype(bf)
    m = np.zeros((128, 128), np.float32)
    for hd in range(2):
        for i in range(8):
            m[hd * 64 + i + 8, hd * 64 + i] = -1.0
            m[hd * 64 + i, hd * 64 + i + 8] = 1.0
    c["msw"] = m.astype(bf)
    pos = np.arange(SEQ, dtype=np.float32)
    inv = np.power(np.float32(500000.0), -np.arange(8, dtype=np.float32) / np.float32(8.0)).astype(np.float32)
    ang = pos[None, :] * inv[:, None]
    ct = np.ones((128, SEQ), np.float32)
    stt = np.zeros((128, SEQ), np.float32)
    for hd in range(2):
        for r in range(16):
            ct[hd * 64 + r] = np.cos(ang[r % 8])
            stt[hd * 64 + r] = np.sin(ang[r % 8])
    c["ropeC"], c["ropeS"] = ct, stt
    c["utri"] = np.triu(np.ones((128, 128), np.float32))
    mk_ = np.zeros((128, 4, 512), np.float32)
    kk = np.arange(128)[:, None]
    qq = np.arange(512)[None, :]
    for i in range(4):
        mk_[:, i, :] = np.where(i * 128 + kk <= qq, 0.0, -30000.0)
    c["maskc"] = mk_.reshape(128, 2048).astype(bf)
    bo = np.zeros((16, SEQ), np.float32)
    for n in range(16):
        bo[n, n * 256:(n + 1) * 256] = 1.0
    c["BO"] = bo.astype(bf)
    fo = np.zeros((16, SEQ), np.float32)
    fo[0] = 1.0
    c["FO"] = fo.astype(bf)
    gm = np.zeros((128, 16, 8, 16), np.float32)
    for nv in range(16):
        gm[:, nv, :, nv:] = -1e30
    c["gmask"] = gm.reshape(128, 16 * 128)
    c["hsel"] = np.full((128, 2), float(half), np.float32)
    c["hsel"][:, 1] = 1.0 - float(half)
    return c


CONST_SPECS = [("ident", [128, 128], F32), ("identb", [128, 128], BF16), ("msw", [128, 128], BF16), ("ropeC", [128, SEQ], F32),
               ("ropeS", [128, SEQ], F32), ("utri", [128, 128], F32), ("maskc", [128, 2048], BF16), ("BO", [16, SEQ], BF16),
               ("FO", [16, SEQ], BF16), ("hsel", [128, 2], F32), ("gmask", [128, 2048], F32)]


def build(upto=99, dbg=False, want_out=False):
    nc = bass.Bass("TRN2", target_bir_lowering=False)
    T = {}
    inp = lambda name, shape, dt=F32: nc.dram_tensor(name, shape, dt, kind="ExternalInput").ap()
    T["x"] = inp("x", [SEQ, D])
    T["c"] = inp("c", [1, D])
    T["w_ada"] = inp("w_ada", [2, D, 6 * D])
    T["b_ada"] = inp("b_ada", [1, 2 * 6 * D])
    T["norm_g"] = inp("norm_g", [1, 8 * D])
    T["attn_w_in"] = inp("attn_w_in", [D, 3080])
    T["fox_b_f"] = inp("fox_b_f", [1, 8])
    T["attn_w_out"] = inp("attn_w_out", [D, D])
    T["ffn_w_gate"] = inp("ffn_w_gate", [D, 2816])
    T["ffn_w_up"] = inp("ffn_w_up", [D, 2816])
    T["ffn_w_down"] = inp("ffn_w_down", [2816, D])
    T["lru_w_in"] = inp("lru_w_in", [D, 2560])
    T["lru_conv_w"] = inp("lru_conv_w", [1, 4 * 1280])
    for nm in ("lru_conv_b", "lru_b_a", "lru_b_x", "lru_lambda"):
        T[nm] = inp(nm, [1, 1280])
    T["lru_w_a"] = inp("lru_w_a", [10, 128, 128])
    T["lru_w_x"] = inp("lru_w_x", [10, 128, 128])
    T["lru_w_out"] = inp("lru_w_out", [1280, D])
    T["moe_w_router"] = inp("moe_w_router", [D, 8])
    T["moe_b_router"] = inp("moe_b_router", [1, 8])
    T["moe_w_gate"] = inp("moe_w_gate", [8, D, 3584])
    T["moe_w_up"] = inp("moe_w_up", [8, D, 3584])
    T["moe_w_down"] = inp("moe_w_down", [8, 3584, D])
    for name, shape, dt in CONST_SPECS:
        T[name] = inp(name, shape, dt)
    kind = "ExternalOutput" if dbg else "Internal"
    scr = lambda name, shape, dt=F32: nc.dram_tensor(name, shape, dt, kind=kind).ap()
    R = {}
    R["QT"] = scr("r_QT", [8, 128, SEQ], BF16)
    R["KT"] = scr("r_KT", [8, 128, SEQ], BF16)
    R["V"] = scr("r_V", [NT, 128, 16 * 65], BF16)
    R["NB"] = scr("r_NB", [128, 8 * NT * 8])
    R["RQ"] = scr("r_RQ", [8, SEQ], BF16)
    R["NS"] = scr("r_NS", [16, 8, SEQ], BF16)
    R["O"] = scr("r_O", [SEQ, D], BF16)
    R["x1a"] = scr("r_x1a", [SEQ, D])
    R["x1"] = scr("r_x1", [SEQ, D])
    R["hs"] = scr("r_hs", [10, 128, SEQ])
    R["x2"] = scr("r_x2", [2048, D])
    R["out"] = nc.dram_tensor("out", [2048, D], F32, kind="ExternalOutput").ap()
    bar = nc.dram_tensor("bar_scratch", [2, 16], F32, kind="Internal").ap()
    es = ExitStack()
    S = Sched(nc, es)
    S.bar_src = bar[0:1, :]
    S.bar_dst = bar[1:2, :]
    P = {}
    P["AT"] = es.enter_context(nc.sbuf_tensor("P_AT", [128, 4, 8], F32))
    P["shT"] = es.enter_context(nc.sbuf_tensor("P_shT", [128, 4, 8], F32))
    P["Brep"] = es.enter_context(nc.sbuf_tensor("P_Brep", [128, 4, D], F32))
    C = {}
    C["ident"] = es.enter_context(nc.sbuf_tensor("C_ident", [128, 128], F32))
    C["identb"] = es.enter_context(nc.sbuf_tensor("C_identb", [128, 128], BF16))
    S.dma("sp", lambda: nc.sync.dma_start(out=C["ident"][:], in_=T["ident"]), w=["ident"])
    S.dma("sp", lambda: nc.sync.dma_start(out=C["identb"][:], in_=T["identb"]), w=["identb"])
    S.dma("sp", lambda: nc.sync.dma_start(out=bar[:, :], in_=C["ident"][0:2, 0:16]), r=["ident"])
    stage0(S, nc, es, T, P)
    if upto >= 1:
        stage1(S, nc, T, P, C, R)
    if upto >= 2:
        stage2(S, nc, T, P, C, R)
    if upto >= 3:
        stage3a(S, nc, T, P, C, R)
        ffn = [(T["ffn_w_gate"], T["ffn_w_up"], T["ffn_w_down"])]
        ffn_pass(S, nc, T, P, C, "f0a_", R["x1a"][0:2048, :], R["x1"][0:2048, :], 1, ffn, 2816)
        ffn_pass(S, nc, T, P, C, "f0b_", R["x1a"][2048:4096, :], R["x1"][2048:4096, :], 1, ffn, 2816)
    if upto >= 4:
        stage4(S, nc, T, P, C, R)
    if upto >= 5:
        stage5(S, nc, T, P, C, R)
    if upto >= 6:
        experts = [(T["moe_w_gate"][e], T["moe_w_up"][e], T["moe_w_down"][e]) for e in range(NEXP)]
        ffn_pass(S, nc, T, P, C, "moe_", R["x2"], R["out"], 3, experts, 3584, router=(T["moe_w_router"], T["moe_b_router"]))
    es.close()
    print("instructions emitted:", S.nemit, flush=True)
    return nc


def make_in_maps(inputs):
    maps = []
    f = lambda a: np.ascontiguousarray(a, dtype=np.float32)
    for core in range(8):
        b, half = core // 2, core % 2
        m = {"x": f(inputs["x"][b]), "c": f(inputs["c"][b:b + 1]), "w_ada": f(inputs["w_ada"]),
             "b_ada": f(inputs["b_ada"]).reshape(1, -1), "norm_g": f(inputs["norm_g"]).reshape(1, -1),
             "attn_w_in": f(inputs["attn_w_in"][0]), "fox_b_f": f(inputs["fox_b_f"]).reshape(1, 8),
             "attn_w_out": f(inputs["attn_w_out"][0]), "ffn_w_gate": f(inputs["ffn_w_gate"][0]),
             "ffn_w_up": f(inputs["ffn_w_up"][0]), "ffn_w_down": f(inputs["ffn_w_down"][0]),
             "lru_w_in": f(inputs["lru_w_in"][0]), "lru_conv_w": f(inputs["lru_conv_w"][0]).reshape(1, -1),
             "lru_conv_b": f(inputs["lru_conv_b"]).reshape(1, -1), "lru_b_a": f(inputs["lru_b_a"]).reshape(1, -1),
             "lru_b_x": f(inputs["lru_b_x"]).reshape(1, -1), "lru_lambda": f(inputs["lru_lambda"]).reshape(1, -1),
             "lru_w_a": f(inputs["lru_w_a"][0]), "lru_w_x": f(inputs["lru_w_x"][0]), "lru_w_out": f(inputs["lru_w_out"][0]),
             "moe_w_router": f(inputs["moe_w_router"][0]), "moe_b_router": f(inputs["moe_b_router"]).reshape(1, 8),
             "moe_w_gate": f(inputs["moe_w_gate"][0]), "moe_w_up": f(inputs["moe_w_up"][0]), "moe_w_down": f(inputs["moe_w_down"][0])}
        m.update(host_consts(half))
        maps.append(m)
    return maps


def kernel(**inputs):
    nc = build(upto=6, dbg=False)
    maps = make_in_maps(inputs)
    res = run_bass_kernel_spmd(nc, maps, core_ids=list(range(8)))
    out = np.zeros((4, SEQ, D), np.float32)
    for core in range(8):
        b, half = core // 2, core % 2
        out[b, half * 2048:(half + 1) * 2048] = np.asarray(res.results[core]["out"], dtype=np.float32)
    return out
```

```python
import os
from contextlib import ExitStack
import numpy as np
import ml_dtypes
import concourse.bass as bass
import concourse.mybir as mybir
from concourse.bass_utils import run_bass_kernel_spmd

F32 = mybir.dt.float32
BF16 = mybir.dt.bfloat16
I32 = mybir.dt.int32
AF = mybir.ActivationFunctionType
ALU = mybir.AluOpType
AX = mybir.AxisListType

D = 1024
SEQ = 4096
NT = SEQ // 128
NEXP = int(os.environ.get('K_NEXP', '8'))
EPS = 1e-6
ENGS = ["pe", "act", "dve", "pool", "sp"]


class _Op:
    __slots__ = ("eng", "fn", "deps", "dma", "signal", "sig")

    def __init__(self, eng, fn, dma):
        self.eng = eng
        self.fn = fn
        self.deps = []
        self.dma = dma
        self.signal = False
        self.sig = None


class Sched:
    NDMA = 28

    def __init__(self, nc, es):
        self.nc = nc
        self.e = {"pe": nc.tensor, "act": nc.scalar, "dve": nc.vector, "pool": nc.gpsimd, "sp": nc.sync}
        self.sem = {k: es.enter_context(nc.semaphore("sem_" + k)) for k in ENGS}
        self.dsem = [es.enter_context(nc.semaphore("dsem%d" % i)) for i in range(self.NDMA)]
        self.bsem = es.enter_context(nc.semaphore("bsem"))
        self.cnt = {k: 0 for k in ENGS}
        self.dtot = [0] * self.NDMA
        self.dnext = 0
        self.NSW = 8
        self.dnext_sw = 0
        self.btot = 0
        self.waited = {k: {} for k in ENGS}
        self.ops = []
        self.lastw = {}
        self.readers = {}
        self.nemit = 0
        self._cap = []
        self.bar_src = None
        self.bar_dst = None

    def _rec(self, eng, fn, r, w, dma):
        op = _Op(eng, fn, dma)
        deps = []
        for k in r:
            lw = self.lastw.get(k)
            if lw is not None:
                deps.append(lw)
            self.readers.setdefault(k, []).append(op)
        for k in w:
            lw = self.lastw.get(k)
            if lw is not None:
                deps.append(lw)
            for rd in self.readers.get(k, ()):
                if rd is not op:
                    deps.append(rd)
            self.lastw[k] = op
            self.readers[k] = []
        seen = set()
        for d in deps:
            if id(d) in seen or d is op:
                continue
            seen.add(id(d))
            if (not d.dma) and (not dma) and d.eng == "pe" and eng == "pe":
                continue
            d.signal = True
            op.deps.append(d)
        self.ops.append(op)
        return op

    def op(self, eng, fn, r=(), w=()):
        if self._cap:
            self._cap[-1].append((eng, fn, tuple(r), tuple(w), False))
            return None
        return self._rec(eng, fn, r, w, False)

    def dma(self, q, fn, r=(), w=()):
        if self._cap:
            self._cap[-1].append((q, fn, tuple(r), tuple(w), True))
            return None
        return self._rec(q, fn, r, w, True)

    def capture(self, body):
        self._cap.append([])
        try:
            body()
        finally:
            ops = self._cap.pop()
        return ops

    def replay(self, threads):
        threads = [t for t in threads if t]
        if not threads:
            return
        n = max(len(t) for t in threads)
        pos = [0] * len(threads)
        for step in range(1, n + 1):
            for i, t in enumerate(threads):
                tgt = (step * len(t)) // n
                while pos[i] < tgt:
                    if self._cap:
                        self._cap[-1].append(t[pos[i]])
                    else:
                        self._rec(*t[pos[i]])
                    pos[i] += 1

    def _wait(self, eng, sem, val):
        wd = self.waited[eng]
        key = id(sem)
        if wd.get(key, 0) >= val:
            return
        wd[key] = val
        self.e[eng].wait_ge(sem, val)

    def flush(self, barrier=True):
        ops = self.ops
        if barrier:
            last = {}
            for op in ops:
                if not op.dma:
                    last[op.eng] = op
            for op in last.values():
                op.signal = True
        for op in ops:
            for d in op.deps:
                sem, val = d.sig
                self._wait(op.eng, sem, val)
            if op.dma:
                if op.eng == "pool":
                    slot = self.NDMA - self.NSW + self.dnext_sw
                    self.dnext_sw = (self.dnext_sw + 1) % self.NSW
                else:
                    slot = self.dnext
                    self.dnext = (self.dnext + 1) % (self.NDMA - self.NSW)
                if self.dtot[slot] > 0:
                    self._wait(op.eng, self.dsem[slot], self.dtot[slot])
                inst = op.fn()
                self.dtot[slot] += 16
                inst.then_inc(self.dsem[slot], 16)
                op.sig = (self.dsem[slot], self.dtot[slot])
            else:
                inst = op.fn()
                if op.signal:
                    self.cnt[op.eng] += 1
                    inst.then_inc(self.sem[op.eng], 1)
                    op.sig = (self.sem[op.eng], self.cnt[op.eng])
            self.nemit += 1
        self.ops = []
        self.lastw = {}
        self.readers = {}
        if barrier:
            for k in ENGS:
                if k != "sp" and self.cnt[k] > 0:
                    self._wait("sp", self.sem[k], self.cnt[k])
            for s in range(self.NDMA):
                if self.dtot[s] > 0:
                    self._wait("sp", self.dsem[s], self.dtot[s])
            self.btot += 16
            self.nc.sync.dma_start(out=self.bar_dst, in_=self.bar_src).then_inc(self.bsem, 16)
            for k in ENGS:
                self.e[k].wait_ge(self.bsem, self.btot)


def stage0(S, nc, es, T, P):
    st = ExitStack()
    sb = lambda name, shape, dt=F32: st.enter_context(nc.sbuf_tensor(name, shape, dt))
    ps = lambda name, shape, dt=F32: st.enter_context(nc.psum_tensor(name, shape, dt))
    one11 = sb("s0_one", [1, 1])
    ones_row = sb("s0_onesrow", [1, 128])
    ones128 = sb("s0_ones128", [128, 128])
    crow = sb("s0_crow", [1, D])
    grow = sb("s0_grow", [1, 8 * D])
    brow = sb("s0_brow", [1, 2 * 6 * D])
    condT = sb("s0_condT", [128, 8])
    cond_rep = sb("s0_condrep", [128, 8, 128], BF16)
    condTb = sb("s0_condTb", [128, 8], BF16)
    gT = sb("s0_gT", [128, 64])
    modT = sb("s0_modT", [128, 64])
    gpost = sb("s0_gpost", [128, 4, D])
    wsl = [sb("s0_w%d" % i, [128, 8, 512], BF16) for i in range(3)]
    pT = ps("s0_pT", [128, 512])
    pG = ps("s0_pG", [128, 512])
    pM = ps("s0_pM", [128, 512])
    pR = [ps("s0_pR%d" % i, [128, 512]) for i in range(2)]
    V, A, PE_, PO = nc.vector, nc.scalar, nc.tensor, nc.gpsimd

    S.op("pool", lambda: PO.memset(one11[:], 1.0), w=["one11"])
    S.op("pool", lambda: PO.memset(ones_row[:], 1.0), w=["ones_row"])
    S.op("pool", lambda: PO.memset(ones128[:], 1.0), w=["ones128"])
    S.dma("sp", lambda: nc.sync.dma_start(out=crow[:], in_=T["c"]), w=["crow"])
    S.dma("sp", lambda: nc.sync.dma_start(out=grow[:], in_=T["norm_g"]), w=["grow"])
    S.dma("sp", lambda: nc.sync.dma_start(out=brow[:], in_=T["b_ada"]), w=["brow"])
    for sub in range(4):
        L, which = sub // 2, sub % 2
        S.dma("sp", lambda sub=sub, L=L, which=which: nc.sync.dma_start(
            out=gpost[:, sub, :], in_=T["norm_g"][0:1, (L * 4 + 2 * which + 1) * D:(L * 4 + 2 * which + 2) * D].partition_broadcast(128)),
            w=["gpost%d" % sub])
    for kc in range(8):
        S.op("pe", lambda kc=kc: PE_.matmul(pT[:, kc:kc + 1], lhsT=crow[0:1, kc * 128:(kc + 1) * 128], rhs=one11[0:1, 0:1],
                                            start=True, stop=True), r=["crow", "one11"], w=["pT"])
    S.op("act", lambda: A.activation(out=condT[:], in_=pT[:, 0:8], func=AF.Silu), r=["pT"], w=["condT"])
    S.op("dve", lambda: V.tensor_copy(out=condTb[:], in_=condT[:]), r=["condT"], w=["condTb"])
    for kc in range(8):
        S.op("dve", lambda kc=kc: V.tensor_scalar(out=cond_rep[:, kc, :], in0=ones128[:], scalar1=condT[:, kc:kc + 1], scalar2=None,
                                                  op0=ALU.mult), r=["condT", "ones128"], w=["cond_rep"])
    for j in range(64):
        S.op("pe", lambda j=j: PE_.matmul(pG[:, j:j + 1], lhsT=grow[0:1, j * 128:(j + 1) * 128], rhs=one11[0:1, 0:1],
                                          start=True, stop=True), r=["grow", "one11"], w=["pG"])
    S.op("dve", lambda: V.tensor_copy(out=gT[:], in_=pG[:, 0:64]), r=["pG"], w=["gT"])
    w_ada = T["w_ada"]
    tidx = {0: 0, 1: 1, 3: 2, 4: 3}
    ri = 0
    for L in range(2):
        for s in range(12):
            buf = (L * 12 + s) % 3
            grp, half = s // 2, s % 2
            S.dma("pool", lambda L=L, s=s, buf=buf: PO.dma_start(
                out=wsl[buf][:], in_=w_ada[L, :, s * 512:(s + 1) * 512].rearrange("(kc p) n -> p kc n", p=128)),
                w=["wsl%d" % buf])
            boff = L * 6 * D + s * 512
            if grp in (2, 5):
                pr = pR[ri % 2]
                prk = "pR%d" % (ri % 2)
                ri += 1
                for kc in range(8):
                    S.op("pe", lambda kc=kc, buf=buf, pr=pr: PE_.matmul(pr[:, :], lhsT=cond_rep[:, kc, :], rhs=wsl[buf][:, kc, :],
                                                                    start=(kc == 0), stop=False),
                         r=["cond_rep", "wsl%d" % buf], w=[prk])
                S.op("pe", lambda boff=boff, pr=pr: PE_.matmul(pr[:, :], lhsT=ones_row[0:1, :], rhs=brow[0:1, boff:boff + 512],
                                                             start=False, stop=True), r=["ones_row", "brow"], w=[prk])
                sub = 2 * L + (0 if grp == 2 else 1)
                S.op("dve", lambda sub=sub, half=half, pr=pr: V.tensor_tensor(
                    out=P["Brep"][:, sub, half * 512:(half + 1) * 512], in0=pr[:, :], in1=gpost[:, sub, half * 512:(half + 1) * 512],
                    op=ALU.mult), r=[prk, "gpost%d" % sub], w=["Brep"])
            else:
                for q in range(4):
                    col = L * 32 + tidx[grp] * 8 + half * 4 + q
                    for kc in range(8):
                        S.op("pe", lambda kc=kc, buf=buf, q=q, col=col: PE_.matmul(
                            pM[:, col:col + 1], lhsT=wsl[buf][:, kc, q * 128:(q + 1) * 128], rhs=condTb[:, kc:kc + 1],
                            start=(kc == 0), stop=False), r=["condTb", "wsl%d" % buf], w=["pM"])
                    S.op("pe", lambda boff=boff, q=q, col=col: PE_.matmul(
                        pM[:, col:col + 1], lhsT=brow[0:1, boff + q * 128:boff + (q + 1) * 128], rhs=one11[0:1, 0:1],
                        start=False, stop=True), r=["brow", "one11"], w=["pM"])
    S.op("dve", lambda: V.tensor_copy(out=modT[:], in_=pM[:, 0:64]), r=["pM"], w=["modT"])
    for sub in range(4):
        L, which = sub // 2, sub % 2
        sh_c = L * 32 + (0 if which == 0 else 2) * 8
        sc_c = L * 32 + (1 if which == 0 else 3) * 8
        g_c = (L * 4 + 2 * which) * 8
        S.op("dve", lambda sub=sub, sc_c=sc_c, g_c=g_c: V.scalar_tensor_tensor(
            out=P["AT"][:, sub, :], in0=modT[:, sc_c:sc_c + 8], scalar=1.0, in1=gT[:, g_c:g_c + 8], op0=ALU.add, op1=ALU.mult),
            r=["modT", "gT"], w=["AT"])
        S.op("dve", lambda sub=sub, sh_c=sh_c: V.tensor_copy(out=P["shT"][:, sub, :], in_=modT[:, sh_c:sh_c + 8]),
             r=["modT"], w=["shT"])
    S.flush()
    st.close()


class Ring:
    def __init__(self, name, tens):
        self.name, self.tens, self.i = name, tens, 0

    def next(self):
        t = self.tens[self.i % len(self.tens)]
        k = "%s%d" % (self.name, self.i % len(self.tens))
        self.i += 1
        return t, k


def mk(nc, st):
    sb = lambda name, shape, dt=F32: st.enter_context(nc.sbuf_tensor(name, shape, dt))
    ps = lambda name, shape, dt=F32: st.enter_context(nc.psum_tensor(name, shape, dt))
    return sb, ps


def prenorm_tile(S, nc, W, xt, kx, sub, P, C, dst_fn, kdst, want32=None, k32=None):
    V, A, PE_ = nc.vector, nc.scalar, nc.tensor
    s_ = W["i"] % len(W["junk"])
    W["i"] += 1
    junk, ss, rstd, xn = W["junk"][s_], W["ss"][s_], W["rstd"][s_], W["xn"][s_]
    tp = W["tp"][s_ % len(W["tp"])]
    ktp = "pn_tp%d" % (s_ % len(W["tp"]))
    p_ = "pn%d_" % s_
    S.op("act", lambda: A.activation(out=junk[:], in_=xt, func=AF.Square, accum_out=ss[:, 0:1]),
         r=[kx], w=[p_ + "junk", p_ + "ss"])
    S.op("act", lambda: A.activation(out=ss[:, 2:3], in_=ss[:, 0:1], func=AF.Sqrt, scale=1.0 / D, bias=EPS),
         r=[p_ + "ss"], w=[p_ + "ss3"])
    S.op("dve", lambda: V.reciprocal(out=rstd[:, 0:1], in_=ss[:, 2:3]), r=[p_ + "ss3"], w=[p_ + "rstd"])
    S.op("act", lambda: A.activation(out=xn[:], in_=xt, func=AF.Copy, scale=rstd[:, 0:1]),
         r=[kx, p_ + "rstd"], w=[p_ + "xn"])
    for kc in range(8):
        S.op("pe", lambda kc=kc: PE_.transpose(out=tp[:, kc * 128:(kc + 1) * 128], in_=xn[:, kc * 128:(kc + 1) * 128],
                                              identity=C["ident"][:]), r=[p_ + "xn", "ident"], w=[ktp])
    for kc in range(8):
        if want32 is not None:
            S.op("dve", lambda kc=kc: V.tensor_scalar(out=want32(kc), in0=tp[:, kc * 128:(kc + 1) * 128],
                                                      scalar1=P["AT"][:, sub, kc:kc + 1], scalar2=P["shT"][:, sub, kc:kc + 1],
                                                      op0=ALU.mult, op1=ALU.add), r=[ktp, "AT", "shT"], w=[k32 + "_%d" % kc])
            S.op("pool", lambda kc=kc: nc.gpsimd.tensor_copy(out=dst_fn(kc), in_=want32(kc)), r=[k32 + "_%d" % kc], w=[kdst + "_%d" % kc])
        else:
            S.op("dve", lambda kc=kc: V.tensor_scalar(out=dst_fn(kc), in0=tp[:, kc * 128:(kc + 1) * 128],
                                                      scalar1=P["AT"][:, sub, kc:kc + 1], scalar2=P["shT"][:, sub, kc:kc + 1],
                                                      op0=ALU.mult, op1=ALU.add), r=[ktp, "AT", "shT"], w=[kdst + "_%d" % kc])


def prenorm_work(nc, st, pfx, ntp=1, nset=2):
    sb, ps = mk(nc, st)
    return {"i": 0, "tp": [ps(pfx + "tp%d" % i, [128, D]) for i in range(ntp)], "junk": [sb(pfx + "junk%d" % i, [128, D], BF16) for i in range(nset)],
            "ss": [sb(pfx + "ss%d" % i, [128, 4]) for i in range(nset)], "rstd": [sb(pfx + "rstd%d" % i, [128, 1]) for i in range(nset)],
            "xn": [sb(pfx + "xn%d" % i, [128, D]) for i in range(nset)]}


def postnorm_tile(S, nc, W, ysrc, ky, xin, kxin, xout, kxout, sub, P):
    V, A = nc.vector, nc.scalar
    s_ = W["i"] % len(W["junk"])
    W["i"] += 1
    junk, ss, rstd, tmp = W["junk"][s_], W["ss"][s_], W["rstd"][s_], W["tmp"][s_]
    p_ = "po%d_" % s_
    for hf in range(2):
        S.op("act", lambda hf=hf: A.activation(out=junk[:, hf * 512:(hf + 1) * 512], in_=ysrc(hf), func=AF.Square,
                                               accum_out=ss[:, hf:hf + 1]), r=ky, w=[p_ + "junk%d" % hf, p_ + "ss%d" % hf])
    S.op("dve", lambda: V.tensor_tensor(out=ss[:, 2:3], in0=ss[:, 0:1], in1=ss[:, 1:2], op=ALU.add),
         r=[p_ + "ss0", p_ + "ss1"], w=[p_ + "s2"])
    S.op("act", lambda: A.activation(out=ss[:, 4:5], in_=ss[:, 2:3], func=AF.Sqrt, scale=1.0 / D, bias=EPS),
         r=[p_ + "s2"], w=[p_ + "s4"])
    S.op("dve", lambda: V.reciprocal(out=rstd[:, 0:1], in_=ss[:, 4:5]), r=[p_ + "s4"], w=[p_ + "rstd"])
    for hf in range(2):
        S.op("dve", lambda hf=hf: V.scalar_tensor_tensor(out=tmp[:, hf * 512:(hf + 1) * 512], in0=ysrc(hf), scalar=rstd[:, 0:1],
                                                         in1=P["Brep"][:, sub, hf * 512:(hf + 1) * 512], op0=ALU.mult, op1=ALU.mult),
             r=ky + [p_ + "rstd", "Brep"], w=[p_ + "tmp%d" % hf])
        S.op("pool", lambda hf=hf: nc.gpsimd.tensor_tensor(out=xout[:, hf * 512:(hf + 1) * 512], in0=tmp[:, hf * 512:(hf + 1) * 512],
                                                            in1=xin[:, hf * 512:(hf + 1) * 512], op=ALU.add),
             r=[p_ + "tmp%d" % hf, kxin], w=[kxout])


def postnorm_work(nc, st, pfx, nset=2):
    sb, ps = mk(nc, st)
    return {"i": 0, "junk": [sb(pfx + "junk%d" % i, [128, D], BF16) for i in range(nset)],
            "ss": [sb(pfx + "ss%d" % i, [128, 8]) for i in range(nset)], "rstd": [sb(pfx + "rstd%d" % i, [128, 1]) for i in range(nset)],
            "tmp": [sb(pfx + "tmp%d" % i, [128, D]) for i in range(nset)]}


def k8(k):
    return [k + "_%d" % i for i in range(8)]

def stage1(S, nc, T, P, C, R):
    st = ExitStack()
    sb, ps = mk(nc, st)
    V, A, PE_, PO = nc.vector, nc.scalar, nc.tensor, nc.gpsimd
    w_bf = sb("s1_w", [128, 8, 3080], BF16)
    CTt = sb("s1_CT", [128, SEQ])
    STt = sb("s1_ST", [128, SEQ])
    msw = sb("s1_msw", [128, 128], BF16)
    U = sb("s1_U", [128, 128])
    ones128 = sb("s1_ones", [128, 128])
    bfr = sb("s1_bfr", [128, 8])
    gmask = sb("s1_gmask", [128, 16, 128])
    xring = Ring("s1x", [sb("s1_x%d" % i, [128, D]) for i in range(2)])
    hring = Ring("s1h", [sb("s1_h%d" % i, [128, 8, 512], BF16) for i in range(2)])
    Wn = prenorm_work(nc, st, "s1n_")
    qbring = Ring("s1qb", [sb("s1_qb%d" % i, [128, 512], BF16) for i in range(2)])
    qoring = Ring("s1qo", [sb("s1_qo%d" % i, [128, 512], BF16) for i in range(3)])
    t1 = sb("s1_t1", [128, 512])
    t2 = sb("s1_t2", [128, 512])
    qmring = Ring("s1qm", [sb("s1_qm%d" % i, [128, 4, 512], BF16) for i in range(2)])
    ksum = sb("s1_ksum", [128, 4, 16])
    ksum_bf = sb("s1_ksumbf", [128, 4, 32], BF16)
    vring = Ring("s1v", [sb("s1_v%d" % i, [128, 16, 65], BF16) for i in range(2)])
    PL = sb("s1_PL", [128, 32, 8])
    zt = sb("s1_zt", [128, 8])
    ez = sb("s1_ez", [128, 8])
    accs = sb("s1_accs", [128, 8])
    cumpos = sb("s1_cum", [128, 32, 8])
    CG = sb("s1_CG", [128, 8, 8])
    rqt = sb("s1_rqt", [128, 8])
    NB2 = sb("s1_NB2", [128, 8, 32, 8])
    GB = sb("s1_GB", [128, 8, 16])
    top8 = sb("s1_top8", [128, 8, 8])
    sel = sb("s1_sel", [128, 8, 16])
    nsel = sb("s1_nsel", [128, 8, 16], BF16)
    nring = Ring("s1ns", [sb("s1_ns%d" % i, [16, 8, 512], BF16) for i in range(2)])
    rqst = sb("s1_rq", [8, SEQ], BF16)
    pjring = Ring("s1pj", [ps("s1_pj%d" % i, [128, 512]) for i in range(2)])
    psw = ps("s1_psw", [128, 512])
    pm = ps("s1_pm", [128, 512])
    ptr = ps("s1_ptr", [16, 1024], BF16)
    pc = ps("s1_pc", [128, 512])

    w_in = T["attn_w_in"]
    for i in range(4):
        S.dma("pool", lambda i=i: PO.dma_start(out=w_bf[:, :, i * 770:(i + 1) * 770],
                                               in_=w_in[:, i * 770:(i + 1) * 770].rearrange("(kc p) n -> p kc n", p=128)), w=["w_bf"])
    S.dma("sp", lambda: nc.sync.dma_start(out=CTt[:], in_=T["ropeC"]), w=["CT"])
    S.dma("sp", lambda: nc.sync.dma_start(out=STt[:], in_=T["ropeS"]), w=["ST"])
    S.dma("pool", lambda: PO.dma_start(out=msw[:], in_=T["msw"]), w=["msw"])
    S.dma("sp", lambda: nc.sync.dma_start(out=U[:], in_=T["utri"]), w=["U"])
    S.dma("sp", lambda: nc.sync.dma_start(out=bfr[:], in_=T["fox_b_f"].partition_broadcast(128)), w=["bfr"])
    S.dma("sp", lambda: nc.sync.dma_start(out=gmask[:].rearrange("p a b -> p (a b)"), in_=T["gmask"]), w=["gmask"])
    S.op("pool", lambda: PO.memset(ones128[:], 1.0), w=["ones128"])
    S.op("pool", lambda: PO.memset(accs[:], 0.0), w=["accs"])
    S.op("pool", lambda: PO.memset(NB2[:], 0.0), w=["NB2"])
    S.op("pool", lambda: PO.memset(ksum[:], 0.0), w=["ksum"])
    S.op("pool", lambda: PO.memset(ksum_bf[:], 0.0), w=["ksum_bf"])
    for i in range(2):
        S.op("pool", lambda i=i: PO.memset(vring.tens[i][:], 1.0), w=["s1v%d" % i])
    x = T["x"]

    def rope(pj, kp, scale, g, dest, kdest):
        qb, kqb = qbring.next()
        S.op("act", lambda: A.activation(out=qb[:], in_=pj[:], func=AF.Copy, scale=scale), r=[kp], w=[kqb])
        S.op("pe", lambda: PE_.matmul(psw[:], lhsT=msw[:], rhs=qb[:], start=True, stop=True), r=[kqb, "msw"], w=["psw"])
        S.op("dve", lambda: V.tensor_tensor(out=t1[:], in0=qb[:], in1=CTt[:, g * 512:(g + 1) * 512], op=ALU.mult),
             r=[kqb, "CT"], w=["t1"])
        S.op("dve", lambda: V.tensor_tensor(out=t2[:], in0=psw[:], in1=STt[:, g * 512:(g + 1) * 512], op=ALU.mult),
             r=["psw", "ST"], w=["t2"])
        S.op("pool", lambda: PO.tensor_tensor(out=dest, in0=t1[:], in1=t2[:], op=ALU.add), r=["t1", "t2"], w=[kdest])

    SKIP = os.environ.get("S1_SKIP", "")
    NG = int(os.environ.get("S1_NG", "8"))
    ctx = {}

    def pn(g):
        hT, kh = hring.next()
        hkeys = []
        for t in range(4):
            ti = 4 * g + t
            xt, kx = xring.next()
            S.dma("sp", lambda xt=xt, ti=ti: nc.sync.dma_start(out=xt[:], in_=x[ti * 128:(ti + 1) * 128, :]), w=[kx])
            kd = "%st%d" % (kh, t)
            prenorm_tile(S, nc, Wn, xt[:], kx, 0, P, C, lambda kc, hT=hT, t=t: hT[:, kc, t * 128:(t + 1) * 128], kd)
            hkeys += k8(kd)
        ctx[g] = (hT, hkeys)

    def main(g):
        hT, hkeys = ctx[g]
        qm, kqm = qmring.next()
        km, kkm = qmring.next()
        for isk in range(0 if "Q" in SKIP else 2):
            for j in range(8):
                pj, kp = pjring.next()
                c0 = isk * 1024 + j * 128
                for kc in range(8):
                    S.op("pe", lambda pj=pj, kc=kc, c0=c0, hT=hT: PE_.matmul(pj[:], lhsT=w_bf[:, kc, c0:c0 + 128], rhs=hT[:, kc, :],
                                                                         start=(kc == 0), stop=(kc == 7)), r=["w_bf"] + hkeys, w=[kp])
                scale = 1.0 if isk else 0.125
                dst_d = (R["KT"] if isk else R["QT"])[j, :, g * 512:(g + 1) * 512]
                if j >= 4 or "R" in SKIP:
                    qo, kq = qoring.next()
                    S.op("act", lambda qo=qo, pj=pj, scale=scale: A.activation(out=qo[:], in_=pj[:], func=AF.Copy, scale=scale),
                         r=[kp], w=[kq])
                    S.dma("sp", lambda qo=qo, dst_d=dst_d: nc.sync.dma_start(out=dst_d, in_=qo[:]), r=[kq])
                else:
                    buf, kb = (km, kkm) if isk else (qm, kqm)
                    kdst = "%s_j%d" % (kb, j)
                    rope(pj, kp, scale, g, buf[:, j, :], kdst)
                    S.dma("pool", lambda buf=buf, j=j, dst_d=dst_d: PO.dma_start(out=dst_d, in_=buf[:, j, :]), r=[kdst])
                    if isk:
                        S.op("dve", lambda buf=buf, j=j, g=g: V.tensor_reduce(
                            out=ksum[:, j, 2 * g:2 * g + 2], in_=buf[:, j, :].rearrange("p (b t) -> p b t", t=256),
                            axis=AX.X, op=ALU.add), r=[kdst], w=["ksum"])
        S.op("pool", lambda: PO.tensor_copy(out=ksum_bf[0:64, :, 0:16], in_=ksum[0:64, :, :]), r=["ksum"], w=["ksum_bf"])
        S.op("pool", lambda: PO.tensor_copy(out=ksum_bf[64:128, :, 16:32], in_=ksum[64:128, :, :]), r=["ksum"], w=["ksum_bf"])
        for t in range(0 if "V" in SKIP else 4):
            ti = 4 * g + t
            vs, kv = vring.next()
            for hf in range(2):
                pj, kp = pjring.next()
                for kc in range(8):
                    S.op("pe", lambda pj=pj, kc=kc, hf=hf, hT=hT, t=t: PE_.matmul(
                        pj[:], lhsT=hT[:, kc, t * 128:(t + 1) * 128], rhs=w_bf[:, kc, 2048 + hf * 512:2048 + (hf + 1) * 512],
                        start=(kc == 0), stop=(kc == 7)), r=["w_bf"] + hkeys, w=[kp])
                eng = "act" if hf == 0 else "dve"
                if hf == 0:
                    S.op("act", lambda pj=pj, vs=vs, hf=hf: A.activation(out=vs[:, hf * 8:(hf + 1) * 8, 0:64],
                                                                         in_=pj[:].rearrange("p (h d) -> p h d", d=64), func=AF.Copy),
                         r=[kp], w=[kv + "_h%d" % hf])
                else:
                    S.op("dve", lambda pj=pj, vs=vs, hf=hf: V.tensor_copy(out=vs[:, hf * 8:(hf + 1) * 8, 0:64],
                                                                          in_=pj[:].rearrange("p (h d) -> p h d", d=64)),
                         r=[kp], w=[kv + "_h%d" % hf])
            S.dma("sp", lambda vs=vs, ti=ti: nc.sync.dma_start(out=R["V"][ti], in_=vs[:].rearrange("p h d -> p (h d)")),
                  r=[kv + "_h0", kv + "_h1"], w=[kv])
            for kc in range(8):
                S.op("pe", lambda kc=kc, hT=hT, t=t: PE_.matmul(pm[:, t * 8:(t + 1) * 8], lhsT=hT[:, kc, t * 128:(t + 1) * 128],
                                                               rhs=w_bf[:, kc, 3072:3080], start=(kc == 0), stop=(kc == 7)),
                     r=["w_bf"] + hkeys, w=["pm"])
            S.op("dve", lambda t=t: V.tensor_tensor(out=zt[:], in0=pm[:, t * 8:(t + 1) * 8], in1=bfr[:], op=ALU.add),
                 r=["pm", "bfr"], w=["zt"])
            S.op("act", lambda: A.activation(out=ez[:], in_=zt[:], func=AF.Exp, scale=-1.0), r=["zt"], w=["ez"])
            S.op("act", lambda ti=ti: A.activation(out=PL[:, ti, :], in_=ez[:], func=AF.Ln, bias=1.0), r=["ez"], w=["PL"])
        ns, kns = nring.next()
        if "G" in SKIP:
            return
        for t in range(4):
            ti = 4 * g + t
            nv = ti // 2
            for j in range(4):
                S.op("pe", lambda j=j, t=t, qm=qm: PE_.matmul(
                    pm[:, 64 + j * 32:64 + j * 32 + 32], lhsT=qm[:, j, t * 128:(t + 1) * 128],
                    rhs=ksum_bf[:, j, :], start=True, stop=True),
                    r=["ksum_bf", "%s_j%d" % (kqm, j)], w=["pm"])
            S.op("dve", lambda nv=nv: V.tensor_tensor(out=GB[:].rearrange("p h n -> p (h n)"), in0=pm[:, 64:192],
                                                      in1=gmask[:, nv, :], op=ALU.add), r=["pm", "gmask"], w=["GB"])
            for h in range(8):
                S.op("dve", lambda h=h: V.max(out=top8[:, h, :], in_=GB[:, h, :]), r=["GB"], w=["top8"])
            for h in range(8):
                S.op("dve", lambda h=h: V.tensor_scalar(out=sel[:, h, :], in0=GB[:, h, :], scalar1=top8[:, h, 2:3], scalar2=None,
                                                        op0=ALU.is_ge), r=["GB", "top8"], w=["sel"])
            S.op("dve", lambda nv=nv: V.memset(sel[:, :, nv:nv + 1], 1.0), w=["sel"])
            S.op("dve", lambda: V.tensor_scalar(out=nsel[:], in0=sel[:], scalar1=-1.0, scalar2=30000.0, op0=ALU.add, op1=ALU.mult),
                 r=["sel"], w=["nsel"])
            for h in range(8):
                S.op("pe", lambda h=h: PE_.transpose(out=ptr[0:16, h * 128:(h + 1) * 128], in_=nsel[:, h, :], identity=C["identb"][:]),
                     r=["nsel", "identb"], w=["ptr"])
            S.op("act", lambda ns=ns, t=t: A.activation(out=ns[:, :, t * 128:(t + 1) * 128],
                                                        in_=ptr[0:16, :].rearrange("p (h q) -> p h q", q=128), func=AF.Copy),
                 r=["ptr"], w=[kns])
        S.dma("sp", lambda ns=ns, g=g: nc.sync.dma_start(out=R["NS"][:, :, g * 512:(g + 1) * 512], in_=ns[:]), r=[kns])
    def cum(G):
        for ti in range(4 * G, 4 * G + 4):
            if ti % 4 == 0:
                S.op("pe", lambda: PE_.matmul(pc[:, 8:16], lhsT=ones128[:], rhs=accs[:], start=True, stop=True),
                     r=["ones128", "accs"], w=["pc"])
                S.op("dve", lambda: V.tensor_copy(out=CG[:, G, :], in_=pc[:, 8:16]), r=["pc"], w=["CG"])
            S.op("pe", lambda ti=ti: PE_.matmul(pc[:, 0:8], lhsT=U[:], rhs=PL[:, ti, :], start=True, stop=False),
                 r=["U", "PL"], w=["pc"])
            S.op("pe", lambda: PE_.matmul(pc[:, 0:8], lhsT=ones128[:], rhs=accs[:], start=False, stop=True),
                 r=["ones128", "accs"], w=["pc"])
            S.op("dve", lambda ti=ti: V.tensor_copy(out=cumpos[:, ti, :], in_=pc[:, 0:8]), r=["pc"], w=["cumpos"])
            S.op("dve", lambda ti=ti: V.tensor_tensor(out=accs[:], in0=accs[:], in1=PL[:, ti, :], op=ALU.add), r=["accs", "PL"], w=["accs"])
            S.op("dve", lambda ti=ti: V.tensor_tensor(out=rqt[:], in0=cumpos[:, ti, :], in1=CG[:, G, :], op=ALU.subtract),
                 r=["cumpos", "CG"], w=["rqt"])
            S.op("pe", lambda: PE_.transpose(out=pc[0:8, 128:256], in_=rqt[:], identity=C["ident"][:]), r=["rqt", "ident"], w=["pc"])
            S.op("act", lambda ti=ti: A.activation(out=rqst[:, ti * 128:(ti + 1) * 128], in_=pc[0:8, 128:256], func=AF.Copy, scale=-1.0),
                 r=["pc"], w=["rqst"])
        for kt in range(4 * G + 4):
            S.op("dve", lambda kt=kt: V.tensor_tensor(out=NB2[:, G, kt, :], in0=cumpos[:, kt, :], in1=CG[:, G, :], op=ALU.subtract),
                 r=["cumpos", "CG"], w=["NB2"])

    S.replay([S.capture(lambda: pn(0))])
    for g in range(NG):
        tm = S.capture(lambda: main(g))
        tn = S.capture(lambda: pn(g + 1)) if g + 1 < NG else []
        tc = S.capture(lambda: cum(g - 1)) if g >= 1 else []
        S.replay([tm, tn, tc])
    S.replay([S.capture(lambda: cum(NG - 1))])
    S.dma("sp", lambda: nc.sync.dma_start(out=R["NB"], in_=NB2[:].rearrange("p g t h -> p (g t h)")), r=["NB2"])
    S.dma("sp", lambda: nc.sync.dma_start(out=R["RQ"], in_=rqst[:]), r=["rqst"])
    S.flush()
    st.close()

def stage2(S, nc, T, P, C, R):
    st = ExitStack()
    sb, ps = mk(nc, st)
    V, A, PE_, PO = nc.vector, nc.scalar, nc.tensor, nc.gpsimd
    qaring = Ring("s2qa", [sb("s2_qa%d" % i, [80, SEQ], BF16) for i in range(2)])
    karing = Ring("s2ka", [sb("s2_ka%d" % i, [80, SEQ], BF16) for i in range(2)])
    vhring = Ring("s2vh", [sb("s2_vh%d" % i, [128, NT, 65], BF16) for i in range(2)])
    NB = sb("s2_nb", [128, 8, NT, 8])
    maskc = sb("s2_mask", [128, 4, 512], BF16)
    ptring = Ring("s2pt", [sb("s2_pt%d" % i, [128, 512], BF16) for i in range(4)])
    Oall = sb("s2_oall", [128, NT, D], BF16)
    osring = Ring("s2os", [sb("s2_os%d" % i, [65, 512]) for i in range(2)])
    rcring = Ring("s2rc", [sb("s2_rc%d" % i, [128, 4]) for i in range(2)])
    psring = Ring("s2ps", [ps("s2_ps%d" % i, [128, 512]) for i in range(4)])
    poring = Ring("s2po", [ps("s2_po%d" % i, [128, 512]) for i in range(2)])
    ptring2 = Ring("s2pq", [ps("s2_pq%d" % i, [128, 512]) for i in range(2)])

    S.dma("sp", lambda: nc.sync.dma_start(out=NB[:].rearrange("p g t h -> p (g t h)"), in_=R["NB"]), w=["NB"])
    S.dma("pool", lambda: PO.dma_start(out=maskc[:].rearrange("p a b -> p (a b)"), in_=T["maskc"]), w=["maskc"])
    for i in range(2):
        S.op("pool", lambda i=i: PO.memset(qaring.tens[i][64:80, :], 0.0), w=["s2qa%d" % i])

    def load_head(h):
        j, r0 = h // 2, (h % 2) * 64
        qa, kqa = qaring.next()
        ka, kka = karing.next()
        vh, kvh = vhring.next()
        S.dma("sp", lambda: nc.sync.dma_start(out=qa[0:64, :], in_=R["QT"][j, r0:r0 + 64, :]), w=[kqa])
        S.dma("sp", lambda: nc.sync.dma_start(out=ka[0:64, :], in_=R["KT"][j, r0:r0 + 64, :]), w=[kka])
        if h < 8:
            S.dma("sp", lambda: nc.sync.dma_start(out=qa[64:80, :], in_=R["NS"][:, h, :]), w=[kqa])
            S.dma("pool", lambda: PO.dma_start(out=ka[64:80, :], in_=T["BO"]), w=[kka])
        else:
            S.dma("sp", lambda: nc.sync.dma_start(out=qa[64:65, :], in_=R["RQ"][h - 8:h - 7, :]), w=[kqa])
            S.dma("pool", lambda: PO.dma_start(out=ka[64:80, :], in_=T["FO"]), w=[kka])
        for q4 in range(4):
            S.dma("sp", lambda q4=q4: nc.sync.dma_start(
                out=vh[:, q4 * 8:(q4 + 1) * 8, :],
                in_=R["V"][q4 * 8:(q4 + 1) * 8].rearrange("t p c -> p t c")[:, :, h * 65:(h + 1) * 65]), w=[kvh])
        return (qa, kqa, ka, kka, vh, kvh)

    units = []
    for h in range(16):
        for G in range(8):
            n = 4 * G + 4
            for kt in range(n):
                units.append((h, G, kt, n))
    heads = {0: load_head(0)}
    LAG = 3
    lagq = []
    pending = []

    def emit_pv(u):
        h, G, kt, n, pt, kpt, po, kpo, c0 = u
        qa, kqa, ka, kka, vh, kvh = heads[h]
        S.op("pe", lambda: PE_.matmul(po[0:65, c0:512], lhsT=vh[:, kt, :], rhs=pt[:, c0:512], start=(kt == 0), stop=(kt == n - 1)),
             r=[kvh, kpt], w=[kpo])
        if kt == n - 1:
            osb, kos = osring.next()
            rc, krc = rcring.next()
            pq, kpq = ptring2.next()
            S.op("dve", lambda: V.tensor_copy(out=osb[:], in_=po[0:65, :]), r=[kpo], w=[kos])

            def epi():
                for i in range(4):
                    S.op("pe", lambda i=i: PE_.transpose(out=pq[:, i * 65:(i + 1) * 65], in_=osb[0:65, i * 128:(i + 1) * 128],
                                                        identity=C["ident"][0:65, 0:65]), r=[kos, "ident"], w=[kpq])
                S.op("dve", lambda: V.reciprocal(out=rc[:], in_=pq[:, 0:260].rearrange("p (i c) -> p i c", c=65)[:, :, 64]),
                     r=[kpq], w=[krc])
                for i in range(4):
                    dst = Oall[:, 4 * G + i, h * 64:(h + 1) * 64]
                    if False:
                        S.op("act", lambda i=i, dst=dst: A.activation(out=dst, in_=pq[:, i * 65:i * 65 + 64], func=AF.Copy,
                                                                      scale=rc[:, i:i + 1]), r=[kpq, krc], w=["Oall"])
                    else:
                        S.op("dve", lambda i=i, dst=dst: V.tensor_scalar(out=dst, in0=pq[:, i * 65:i * 65 + 64], scalar1=rc[:, i:i + 1],
                                                                         scalar2=None, op0=ALU.mult), r=[kpq, krc], w=["Oall"])
            pending.append(epi)

    for idx, (h, G, kt, n) in enumerate(units):
        qa, kqa, ka, kka, vh, kvh = heads[h]
        pss, kps = psring.next()
        diag = kt >= 4 * G
        c0 = (kt - 4 * G) * 128 if diag else 0
        S.op("pe", lambda pss=pss, ka=ka, qa=qa, kt=kt, G=G, diag=diag, c0=c0: PE_.matmul(
            pss[:, c0:512], lhsT=ka[0:80, kt * 128:(kt + 1) * 128], rhs=qa[0:80, G * 512 + c0:(G + 1) * 512], start=True, stop=(not diag)),
            r=[kka, kqa], w=[kps])
        if diag:
            S.op("pe", lambda pss=pss, kt=kt, G=G, c0=c0: PE_.matmul(pss[:, c0:512], lhsT=C["identb"][:], rhs=maskc[:, kt - 4 * G, c0:512],
                                                                    start=False, stop=True), r=["identb", "maskc"], w=[kps])
        while pending:
            pending.pop(0)()
        pt, kpt = ptring.next()
        if h >= 8:
            S.op("act", lambda pt=pt, pss=pss, kt=kt, h=h, G=G, c0=c0: A.activation(out=pt[:, c0:512], in_=pss[:, c0:512], func=AF.Exp,
                                                                                    bias=NB[:, G, kt, h - 8:h - 7]), r=[kps, "NB"], w=[kpt])
        else:
            S.op("act", lambda pt=pt, pss=pss, c0=c0: A.activation(out=pt[:, c0:512], in_=pss[:, c0:512], func=AF.Exp), r=[kps], w=[kpt])
        if kt == 0:
            po, kpo = poring.next()
            cur_po = (po, kpo)
        lagq.append((h, G, kt, n, pt, kpt, cur_po[0], cur_po[1], c0))
        if len(lagq) > LAG:
            emit_pv(lagq.pop(0))
        if G == 0 and kt == LAG and h + 1 < 16:
            heads[h + 1] = load_head(h + 1)
    while lagq:
        emit_pv(lagq.pop(0))
    while pending:
        pending.pop(0)()
    Od = R["O"].rearrange("(t p) c -> p t c", p=128)
    for q4 in range(4):
        S.dma("sp", lambda q4=q4: nc.sync.dma_start(out=Od[:, q4 * 8:(q4 + 1) * 8, :], in_=Oall[:, q4 * 8:(q4 + 1) * 8, :]), r=["Oall"])
    S.flush()
    st.close()

def stage3a(S, nc, T, P, C, R):
    st = ExitStack()
    sb, ps = mk(nc, st)
    V, A, PE_, PO = nc.vector, nc.scalar, nc.tensor, nc.gpsimd
    wo = sb("s3_wo", [128, 8, D], BF16)
    oring = Ring("s3o", [sb("s3_o%d" % i, [128, D], BF16) for i in range(4)])
    otring = Ring("s3ot", [sb("s3_ot%d" % i, [128, 8, 128], BF16) for i in range(2)])
    xring = Ring("s3x", [sb("s3_x%d" % i, [128, D]) for i in range(4)])
    xoring = Ring("s3xo", [sb("s3_xo%d" % i, [128, D]) for i in range(4)])
    Wp = postnorm_work(nc, st, "s3p_")
    ptr = Ring("s3ptr", [ps("s3_ptr%d" % i, [128, 1024], BF16) for i in range(2)])
    pyr = Ring("s3py", [ps("s3_py%d" % i, [128, 512]) for i in range(4)])
    for i in range(2):
        S.dma("pool", lambda i=i: PO.dma_start(out=wo[:, :, i * 512:(i + 1) * 512],
                                               in_=T["attn_w_out"][:, i * 512:(i + 1) * 512].rearrange("(kc p) n -> p kc n", p=128)),
              w=["wo"])
    def tile(ti):
        ot_, ko = oring.next()
        S.dma("sp", lambda ot_=ot_, ti=ti: nc.sync.dma_start(out=ot_[:], in_=R["O"][ti * 128:(ti + 1) * 128, :]), w=[ko])
        xt, kx = xring.next()
        S.dma("sp", lambda xt=xt, ti=ti: nc.sync.dma_start(out=xt[:], in_=T["x"][ti * 128:(ti + 1) * 128, :]), w=[kx])
        pt, kpt = ptr.next()
        for kc in range(8):
            S.op("pe", lambda pt=pt, ot_=ot_, kc=kc: PE_.transpose(out=pt[:, kc * 128:(kc + 1) * 128], in_=ot_[:, kc * 128:(kc + 1) * 128],
                                                                  identity=C["identb"][:]), r=[ko, "identb"], w=[kpt])
        oT, koT = otring.next()
        S.op("dve", lambda oT=oT, pt=pt: V.tensor_copy(out=oT[:, 0:4, :], in_=pt[:, 0:512].rearrange("p (k q) -> p k q", q=128)),
             r=[kpt], w=[koT + "a"])
        S.op("dve", lambda oT=oT, pt=pt: V.tensor_copy(out=oT[:, 4:8, :], in_=pt[:, 512:1024].rearrange("p (k q) -> p k q", q=128)),
             r=[kpt], w=[koT + "b"])
        pys = []
        for hf in range(2):
            py, kpy = pyr.next()
            pys.append((py, kpy))
            for kc in range(8):
                S.op("pe", lambda py=py, oT=oT, kc=kc, hf=hf: PE_.matmul(py[:], lhsT=oT[:, kc, :], rhs=wo[:, kc, hf * 512:(hf + 1) * 512],
                                                                      start=(kc == 0), stop=(kc == 7)), r=[koT + "a", koT + "b", "wo"], w=[kpy])
        xo, kxo = xoring.next()
        postnorm_tile(S, nc, Wp, lambda hf, pys=pys: pys[hf][0][:], [pys[0][1], pys[1][1]], xt, kx, xo, kxo, 0, P)
        S.dma("pool", lambda xo=xo, ti=ti: PO.dma_start(out=R["x1a"][ti * 128:(ti + 1) * 128, :], in_=xo[:]), r=[kxo])

    for ti in range(0, NT, 2):
        ta = S.capture(lambda: tile(ti))
        tb = S.capture(lambda: tile(ti + 1))
        S.replay([ta, tb])
    S.flush()
    st.close()


def ffn_pass(S, nc, T, P, C, pfx, x_src, x_dst, sub, experts, F, router=None):
    V, A, PE_, PO = nc.vector, nc.scalar, nc.tensor, nc.gpsimd
    NTL = 16
    st0 = ExitStack()
    sb0, _ = mk(nc, st0)
    hT = sb0(pfx + "hT", [128, 8, NTL * 128], BF16)
    acc = sb0(pfx + "acc", [128, NTL, D])
    G = sb0(pfx + "G", [128, NTL, 8]) if router is not None else None
    st = ExitStack()
    sb, ps = mk(nc, st)
    NW = 4 if router is None else 2
    xring = Ring(pfx + "x", [sb(pfx + "x%d" % i, [128, D]) for i in range(2 * NW)])
    Wn = prenorm_work(nc, st, pfx + "n_", ntp=NW, nset=NW)
    if router is not None:
        RT = [dict(h32=sb(pfx + "h32_%d" % i, [128, 8, 128]), lg=sb(pfx + "lg%d" % i, [128, 8]), t8=sb(pfx + "t8%d" % i, [128, 8]),
                   nmx=sb(pfx + "nmx%d" % i, [128, 1]), msk=sb(pfx + "msk%d" % i, [128, 8]), ex=sb(pfx + "ex%d" % i, [128, 8]),
                   den=sb(pfx + "den%d" % i, [128, 2]), pl=ps(pfx + "pl%d" % i, [128, 512])) for i in range(2)]
        wr = sb(pfx + "wr", [128, 8, 8])
        br = sb(pfx + "br", [128, 8])
        S.dma("sp", lambda: nc.sync.dma_start(out=wr[:], in_=router[0].rearrange("(kc p) e -> p kc e", p=128)), w=["wr"])
        S.dma("sp", lambda: nc.sync.dma_start(out=br[:], in_=router[1].partition_broadcast(128)), w=["br"])

    def tile_a(t):
        xt, kx = xring.next()
        S.dma("sp", lambda: nc.sync.dma_start(out=xt[:], in_=x_src[t * 128:(t + 1) * 128, :]), w=[kx])
        kd = pfx + "hTt%d" % t
        if router is None:
            prenorm_tile(S, nc, Wn, xt[:], kx, sub, P, C, lambda kc: hT[:, kc, t * 128:(t + 1) * 128], kd)
            return
        q = t % 2
        rt = RT[q]
        h32, lg, t8, nmx, msk, ex, den, pl = (rt[k] for k in ("h32", "lg", "t8", "nmx", "msk", "ex", "den", "pl"))
        sfx = "_r%d" % q
        prenorm_tile(S, nc, Wn, xt[:], kx, sub, P, C, lambda kc: hT[:, kc, t * 128:(t + 1) * 128], kd,
                     want32=lambda kc: h32[:, kc, :], k32=pfx + "h32" + sfx)
        for kc in range(8):
            S.op("pe", lambda kc=kc: PE_.matmul(pl[:, 0:8], lhsT=h32[:, kc, :], rhs=wr[:, kc, :], start=(kc == 0), stop=(kc == 7)),
                 r=[pfx + "h32" + sfx + "_%d" % kc, "wr"], w=["pl" + sfx])
        S.op("dve", lambda: V.tensor_tensor(out=lg[:], in0=pl[:, 0:8], in1=br[:], op=ALU.add), r=["pl" + sfx, "br"], w=["lg" + sfx])
        S.op("dve", lambda: V.max(out=t8[:], in_=lg[:]), r=["lg" + sfx], w=["t8" + sfx])
        S.op("dve", lambda: V.tensor_scalar(out=nmx[:], in0=t8[:, 0:1], scalar1=-1.0, scalar2=None, op0=ALU.mult), r=["t8" + sfx], w=["nmx" + sfx])
        S.op("dve", lambda: V.tensor_scalar(out=msk[:], in0=lg[:], scalar1=t8[:, 1:2], scalar2=None, op0=ALU.is_ge),
             r=["lg" + sfx, "t8" + sfx], w=["msk" + sfx])
        S.op("act", lambda: A.activation(out=ex[:], in_=lg[:], func=AF.Exp, bias=nmx[:, 0:1]), r=["lg" + sfx, "nmx" + sfx], w=["ex" + sfx])
        S.op("dve", lambda: V.tensor_tensor(out=ex[:], in0=ex[:], in1=msk[:], op=ALU.mult), r=["ex" + sfx, "msk" + sfx], w=["ex" + sfx])
        S.op("dve", lambda: V.tensor_reduce(out=den[:, 0:1], in_=ex[:], axis=AX.X, op=ALU.add), r=["ex" + sfx], w=["den" + sfx])
        S.op("dve", lambda: V.reciprocal(out=den[:, 1:2], in_=den[:, 0:1]), r=["den" + sfx], w=["den2" + sfx])
        S.op("dve", lambda: V.tensor_scalar(out=G[:, t, :], in0=ex[:], scalar1=den[:, 1:2], scalar2=None, op0=ALU.mult),
             r=["ex" + sfx, "den2" + sfx], w=["G%d" % t])

    NI = 4 if router is None else 2
    for t in range(0, NTL, NI):
        S.replay([S.capture(lambda i=i: tile_a(t + i)) for i in range(NI)])
    S.flush()
    st.close()
    st = ExitStack()
    sb, ps = mk(nc, st)
    wgr = Ring(pfx + "wg", [sb(pfx + "wg%d" % i, [128, 8, 512], BF16) for i in range(2)])
    wur = Ring(pfx + "wu", [sb(pfx + "wu%d" % i, [128, 8, 512], BF16) for i in range(2)])
    wdr = Ring(pfx + "wd", [sb(pfx + "wd%d" % i, [128, 4, D], BF16) for i in range(2)])
    h1 = sb(pfx + "h1", [128, 4, NTL * 128], BF16)
    sgr = Ring(pfx + "sg", [sb(pfx + "sg%d" % i, [128, 512]) for i in range(2)])
    pgr = Ring(pfx + "pg", [ps(pfx + "pg%d" % i, [128, 512]) for i in range(2)])
    pur = Ring(pfx + "pu", [ps(pfx + "pu%d" % i, [128, 512]) for i in range(2)])
    pdr = Ring(pfx + "pd", [ps(pfx + "pd%d" % i, [128, 512]) for i in range(4)])
    first = True
    for e, (Wg, Wu, Wd) in enumerate(experts):
        nfg = (F + 511) // 512
        for fg in range(nfg):
            f0 = fg * 512
            nch = min(4, (F - f0) // 128)
            wg, kwg = wgr.next()
            wu, kwu = wur.next()
            wd, kwd = wdr.next()
            S.dma("pool", lambda wg=wg, Wg=Wg, f0=f0, nch=nch: PO.dma_start(
                out=wg[:, :, 0:nch * 128], in_=Wg[:, f0:f0 + nch * 128].rearrange("(kc p) n -> p kc n", p=128)), w=[kwg])
            S.dma("pool", lambda wu=wu, Wu=Wu, f0=f0, nch=nch: PO.dma_start(
                out=wu[:, :, 0:nch * 128], in_=Wu[:, f0:f0 + nch * 128].rearrange("(kc p) n -> p kc n", p=128)), w=[kwu])
            S.dma("pool", lambda wd=wd, Wd=Wd, f0=f0, nch=nch: PO.dma_start(
                out=wd[:, 0:nch, :], in_=Wd[f0:f0 + nch * 128, :].rearrange("(c p) n -> p c n", p=128)), w=[kwd])
            for c in range(nch):
                for tg in range(4):
                    pg_, kpg = pgr.next()
                    pu_, kpu = pur.next()
                    for kc in range(8):
                        S.op("pe", lambda pg_=pg_, wg=wg, kc=kc, c=c, tg=tg: PE_.matmul(
                            pg_[:], lhsT=wg[:, kc, c * 128:(c + 1) * 128], rhs=hT[:, kc, tg * 512:(tg + 1) * 512],
                            start=(kc == 0), stop=(kc == 7)), r=[kwg], w=[kpg])
                    for kc in range(8):
                        S.op("pe", lambda pu_=pu_, wu=wu, kc=kc, c=c, tg=tg: PE_.matmul(
                            pu_[:], lhsT=wu[:, kc, c * 128:(c + 1) * 128], rhs=hT[:, kc, tg * 512:(tg + 1) * 512],
                            start=(kc == 0), stop=(kc == 7)), r=[kwu], w=[kpu])
                    sg, ksg = sgr.next()
                    S.op("act", lambda sg=sg, pg_=pg_: A.activation(out=sg[:], in_=pg_[:], func=AF.Silu), r=[kpg], w=[ksg])
                    S.op("dve", lambda sg=sg, pu_=pu_, c=c, tg=tg: V.tensor_tensor(
                        out=h1[:, c, tg * 512:(tg + 1) * 512], in0=pu_[:], in1=sg[:], op=ALU.mult), r=[kpu, ksg],
                        w=[pfx + "h1_%d_%d" % (c, tg)])
            for t in range(NTL):
                for hf in range(2):
                    pd, kpd = pdr.next()
                    for c in range(nch):
                        S.op("pe", lambda pd=pd, wd=wd, c=c, t=t, hf=hf, nch=nch: PE_.matmul(
                            pd[:], lhsT=h1[:, c, t * 128:(t + 1) * 128], rhs=wd[:, c, hf * 512:(hf + 1) * 512],
                            start=(c == 0), stop=(c == nch - 1)), r=[kwd, pfx + "h1_%d_%d" % (c, t // 4)], w=[kpd])
                    dst = acc[:, t, hf * 512:(hf + 1) * 512]
                    ka = pfx + "acc_%d_%d" % (t, hf)
                    if first:
                        if G is None:
                            S.op("dve", lambda pd=pd, dst=dst: V.tensor_copy(out=dst, in_=pd[:]), r=[kpd], w=[ka])
                        else:
                            S.op("dve", lambda pd=pd, dst=dst, t=t, e=e: V.tensor_scalar(
                                out=dst, in0=pd[:], scalar1=G[:, t, e:e + 1], scalar2=None, op0=ALU.mult), r=[kpd], w=[ka])
                    else:
                        if G is None:
                            S.op("dve", lambda pd=pd, dst=dst: V.tensor_tensor(out=dst, in0=pd[:], in1=dst, op=ALU.add), r=[kpd, ka], w=[ka])
                        else:
                            S.op("dve", lambda pd=pd, dst=dst, t=t, e=e: V.scalar_tensor_tensor(
                                out=dst, in0=pd[:], scalar=G[:, t, e:e + 1], in1=dst, op0=ALU.mult, op1=ALU.add), r=[kpd, ka], w=[ka])
            first = False
    S.flush()
    st.close()
    st = ExitStack()
    sb, ps = mk(nc, st)
    xring = Ring(pfx + "cx", [sb(pfx + "cx%d" % i, [128, D]) for i in range(8)])
    xoring = Ring(pfx + "cxo", [sb(pfx + "cxo%d" % i, [128, D]) for i in range(8)])
    Wp = postnorm_work(nc, st, pfx + "p_", nset=4)
    def tile_c(t):
        xt, kx = xring.next()
        S.dma("sp", lambda xt=xt, t=t: nc.sync.dma_start(out=xt[:], in_=x_src[t * 128:(t + 1) * 128, :]), w=[kx])
        xo, kxo = xoring.next()
        postnorm_tile(S, nc, Wp, lambda hf, t=t: acc[:, t, hf * 512:(hf + 1) * 512], [], xt, kx, xo, kxo, sub, P)
        S.dma("pool", lambda xo=xo, t=t: PO.dma_start(out=x_dst[t * 128:(t + 1) * 128, :], in_=xo[:]), r=[kxo])

    for t in range(0, NTL, 4):
        S.replay([S.capture(lambda i=i: tile_c(t + i)) for i in range(4)])
    S.flush()
    st.close()
    st0.close()

def stage4(S, nc, T, P, C, R):
    st = ExitStack()
    sb, ps = mk(nc, st)
    V, A, PE_, PO = nc.vector, nc.scalar, nc.tensor, nc.gpsimd
    DR = 1280
    wu = sb("s4_wu", [128, 8, DR], BF16)
    wa = sb("s4_wa", [128, 10, 128], BF16)
    wx = sb("s4_wx", [128, 10, 128], BF16)
    prow = sb("s4_prow", [1, 8 * DR])
    one11 = sb("s4_one", [1, 1])
    par = sb("s4_par", [128, 80])
    ex = sb("s4_ex", [128, 10])
    tq = sb("s4_tq", [128, 10])
    nsp8 = sb("s4_nsp8", [128, 10])
    nsp16 = sb("s4_nsp16", [128, 10])
    hcar = sb("s4_hcar", [128, 10])
    hba = sb("s4_hba", [128, 10])
    hbx = sb("s4_hbx", [128, 10])
    h8 = sb("s4_h8", [128, 10])
    h16 = sb("s4_h16", [128, 10])
    xring = Ring("s4x", [sb("s4_x%d" % i, [128, D]) for i in range(2)])
    hring = Ring("s4h", [sb("s4_h%d" % i, [128, 8, 512], BF16) for i in range(2)])
    Wn = prenorm_work(nc, st, "s4n_")
    ub = sb("s4_ub", [128, 10, 515])
    cv_r = Ring("s4cv", [sb("s4_cv%d" % i, [128, 512]) for i in range(4)])
    cvb_r = Ring("s4cvb", [sb("s4_cvb%d" % i, [128, 512], BF16) for i in range(2)])
    rg_r = Ring("s4rg", [sb("s4_rg%d" % i, [128, 512]) for i in range(4)])
    ig_r = Ring("s4ig", [sb("s4_ig%d" % i, [128, 512]) for i in range(4)])
    av_r = Ring("s4av", [sb("s4_av%d" % i, [128, 512]) for i in range(2)])
    a2_r = Ring("s4a2", [sb("s4_a2%d" % i, [128, 512]) for i in range(2)])
    xin_r = Ring("s4xin", [sb("s4_xin%d" % i, [128, 512]) for i in range(2)])
    hsr = Ring("s4hs", [sb("s4_hs%d" % i, [128, 512]) for i in range(2)])
    pur = Ring("s4pu", [ps("s4_pu%d" % i, [128, 512]) for i in range(2)])
    pr_r = Ring("s4pr", [ps("s4_pr%d" % i, [128, 512]) for i in range(2)])
    px_r = Ring("s4px", [ps("s4_px%d" % i, [128, 512]) for i in range(2)])
    pp = pr_r.tens[0]

    w_in = T["lru_w_in"]
    for i in range(2):
        S.dma("pool", lambda i=i: PO.dma_start(out=wu[:, :, i * 640:(i + 1) * 640],
                                               in_=w_in[:, DR + i * 640:DR + (i + 1) * 640].rearrange("(kc p) n -> p kc n", p=128)), w=["wu"])
    S.dma("pool", lambda: PO.dma_start(out=wa[:], in_=T["lru_w_a"].rearrange("n c d -> c n d")), w=["wa"])
    S.dma("pool", lambda: PO.dma_start(out=wx[:], in_=T["lru_w_x"].rearrange("n c d -> c n d")), w=["wx"])
    S.dma("sp", lambda: nc.sync.dma_start(out=prow[0:1, 0:4 * DR], in_=T["lru_conv_w"]), w=["prow"])
    for r, nm in enumerate(["lru_conv_b", "lru_b_a", "lru_b_x", "lru_lambda"]):
        S.dma("sp", lambda r=r, nm=nm: nc.sync.dma_start(out=prow[0:1, (4 + r) * DR:(5 + r) * DR], in_=T[nm]), w=["prow"])
    S.op("pool", lambda: PO.memset(one11[:], 1.0), w=["one11"])
    S.op("pool", lambda: PO.memset(ub[:], 0.0), w=["ub"])
    S.op("pool", lambda: PO.memset(hcar[:], 0.0), w=["hcar%d" % c for c in range(10)])
    for r in range(8):
        for c in range(10):
            S.op("pe", lambda r=r, c=c: PE_.matmul(pp[:, r * 10 + c:r * 10 + c + 1], lhsT=prow[0:1, r * DR + c * 128:r * DR + (c + 1) * 128],
                                                  rhs=one11[0:1, 0:1], start=True, stop=True), r=["prow", "one11"], w=["s4pr0"])
    S.op("dve", lambda: V.tensor_copy(out=par[:], in_=pp[:, 0:80]), r=["s4pr0"], w=["par"])
    S.op("act", lambda: A.activation(out=ex[:], in_=par[:, 70:80], func=AF.Exp, scale=-1.0), r=["par"], w=["ex"])
    S.op("dve", lambda: V.tensor_scalar(out=tq[:], in0=ex[:], scalar1=-1.0 / 3.0, scalar2=0.5, op0=ALU.mult, op1=ALU.add), r=["ex"], w=["tq"])
    S.op("dve", lambda: V.tensor_tensor(out=tq[:], in0=tq[:], in1=ex[:], op=ALU.mult), r=["tq", "ex"], w=["tq"])
    S.op("dve", lambda: V.tensor_scalar(out=tq[:], in0=tq[:], scalar1=-1.0, scalar2=1.0, op0=ALU.mult, op1=ALU.add), r=["tq"], w=["tq"])
    S.op("dve", lambda: V.tensor_tensor(out=tq[:], in0=tq[:], in1=ex[:], op=ALU.mult), r=["tq", "ex"], w=["tq"])
    S.op("dve", lambda: V.tensor_scalar(out=nsp8[:], in0=tq[:], scalar1=-8.0, scalar2=None, op0=ALU.mult), r=["tq"], w=["nsp8"])
    S.op("dve", lambda: V.tensor_scalar(out=nsp16[:], in0=tq[:], scalar1=-16.0, scalar2=None, op0=ALU.mult), r=["tq"], w=["nsp16"])
    S.op("dve", lambda: V.tensor_scalar(out=h8[:], in0=tq[:], scalar1=-4.0, scalar2=None, op0=ALU.mult), r=["tq"], w=["h8"])
    S.op("dve", lambda: V.tensor_scalar(out=h16[:], in0=tq[:], scalar1=-8.0, scalar2=None, op0=ALU.mult), r=["tq"], w=["h16"])
    S.op("dve", lambda: V.tensor_scalar(out=hba[:], in0=par[:, 50:60], scalar1=0.5, scalar2=None, op0=ALU.mult), r=["par"], w=["hba"])
    S.op("dve", lambda: V.tensor_scalar(out=hbx[:], in0=par[:, 60:70], scalar1=0.5, scalar2=None, op0=ALU.mult), r=["par"], w=["hbx"])
    x1 = R["x1"]
    ctx = {}

    def pn(g):
        hT, kh = hring.next()
        hkeys = []
        for t in range(4):
            ti = 4 * g + t
            xt, kx = xring.next()
            S.dma("sp", lambda xt=xt, ti=ti: nc.sync.dma_start(out=xt[:], in_=x1[ti * 128:(ti + 1) * 128, :]), w=[kx])
            kd = "%st%d" % (kh, t)
            prenorm_tile(S, nc, Wn, xt[:], kx, 2, P, C, lambda kc, hT=hT, t=t: hT[:, kc, t * 128:(t + 1) * 128], kd)
            hkeys += k8(kd)
        ctx[g] = (hT, hkeys)

    def main(g):
        hT, hkeys = ctx[g]

        def stage_a(c):
            pu, kpu = pur.next()
            for kc in range(8):
                S.op("pe", lambda kc=kc: PE_.matmul(pu[:], lhsT=wu[:, kc, c * 128:(c + 1) * 128], rhs=hT[:, kc, :],
                                                    start=(kc == 0), stop=(kc == 7)), r=["wu"] + hkeys, w=[kpu])
            kub = "ub%d" % c
            cv, kcv = cv_r.next()
            cvb, kcvb = cvb_r.next()
            pr, kpr = pr_r.next()
            px, kpx = px_r.next()
            rg, krg = rg_r.next()
            ig, kig = ig_r.next()
            S.op("act", lambda: A.activation(out=ub[:, c, 3:515], in_=pu[:], func=AF.Copy), r=[kpu], w=[kub])
            S.op("dve", lambda: V.tensor_scalar(out=cv[:], in0=ub[:, c, 3:515], scalar1=par[:, 30 + c:31 + c], scalar2=par[:, 40 + c:41 + c],
                                                op0=ALU.mult, op1=ALU.add), r=[kub, "par"], w=[kcv])
            for j in range(3):
                S.op("dve", lambda j=j: V.scalar_tensor_tensor(out=cv[:], in0=ub[:, c, j:j + 512], scalar=par[:, j * 10 + c:j * 10 + c + 1],
                                                               in1=cv[:], op0=ALU.mult, op1=ALU.add), r=[kub, "par", kcv], w=[kcv])
            S.op("pool", lambda: PO.tensor_copy(out=ub[:, c, 0:3], in_=ub[:, c, 512:515]), r=[kub], w=[kub])
            S.op("pool", lambda: PO.tensor_copy(out=cvb[:], in_=cv[:]), r=[kcv], w=[kcvb])
            S.op("pe", lambda: PE_.matmul(pr[:], lhsT=wa[:, c, :], rhs=cvb[:], start=True, stop=True), r=["wa", kcvb], w=[kpr])
            S.op("pe", lambda: PE_.matmul(px[:], lhsT=wx[:, c, :], rhs=cvb[:], start=True, stop=True), r=["wx", kcvb], w=[kpx])
            S.op("act", lambda: A.activation(out=rg[:], in_=pr[:], func=AF.Tanh, scale=0.5, bias=hba[:, c:c + 1]), r=[kpr, "hba"], w=[krg])
            S.op("act", lambda: A.activation(out=ig[:], in_=px[:], func=AF.Tanh, scale=0.5, bias=hbx[:, c:c + 1]), r=[kpx, "hbx"], w=[kig])
            return (c, cv, kcv, rg, krg, ig, kig)

        def stage_b(stt):
            c, cv, kcv, rg, krg, ig, kig = stt
            av, kav = av_r.next()
            a2, ka2 = a2_r.next()
            xin, kxin = xin_r.next()
            S.op("act", lambda: A.activation(out=av[:], in_=rg[:], func=AF.Exp, scale=h8[:, c:c + 1], bias=h8[:, c:c + 1]),
                 r=[krg, "h8"], w=[kav])
            S.op("act", lambda: A.activation(out=a2[:], in_=rg[:], func=AF.Exp, scale=h16[:, c:c + 1], bias=h16[:, c:c + 1]),
                 r=[krg, "h16"], w=[ka2])
            S.op("act", lambda: A.activation(out=a2[:], in_=a2[:], func=AF.Sqrt, scale=-0.25, bias=0.25), r=[ka2], w=[ka2])
            S.op("dve", lambda: V.scalar_tensor_tensor(out=xin[:], in0=ig[:], scalar=1.0, in1=cv[:], op0=ALU.add, op1=ALU.mult),
                 r=[kig, kcv], w=[kxin])
            S.op("dve", lambda: V.tensor_tensor(out=xin[:], in0=xin[:], in1=a2[:], op=ALU.mult), r=[kxin, ka2], w=[kxin])
            hs_, khs = hsr.next()
            khc = "hcar%d" % c
            S.op("dve", lambda: V.tensor_tensor_scan(out=hs_[:], data0=av[:], data1=xin[:], initial=hcar[:, c:c + 1],
                                                     op0=ALU.mult, op1=ALU.add), r=[kav, kxin, khc], w=[khs])
            S.op("dve", lambda: V.tensor_copy(out=hcar[:, c:c + 1], in_=hs_[:, 511:512]), r=[khs], w=[khc])
            S.dma("sp", lambda: nc.sync.dma_start(out=R["hs"][c, :, g * 512:(g + 1) * 512], in_=hs_[:]), r=[khs])

        sts = {}

        def run_a(c):
            sts[c] = stage_a(c)

        S.replay([S.capture(lambda: run_a(0)), S.capture(lambda: run_a(1))])
        for c in range(0, 10, 2):
            thr = [S.capture(lambda: stage_b(sts[c])), S.capture(lambda: stage_b(sts[c + 1]))]
            if c + 2 < 10:
                thr += [S.capture(lambda: run_a(c + 2)), S.capture(lambda: run_a(c + 3))]
            S.replay(thr)

    S.replay([S.capture(lambda: pn(0))])
    for g in range(8):
        tm = S.capture(lambda: main(g))
        tn = S.capture(lambda: pn(g + 1)) if g + 1 < 8 else []
        S.replay([tm, tn])
    S.flush()
    st.close()


def stage5(S, nc, T, P, C, R):
    st = ExitStack()
    sb, ps = mk(nc, st)
    V, A, PE_, PO = nc.vector, nc.scalar, nc.tensor, nc.gpsimd
    DR = 1280
    wg = sb("s5_wg", [128, 8, DR], BF16)
    wo = sb("s5_wo", [128, 10, D], BF16)
    hsel = sb("s5_hsel", [128, 2])
    xar = Ring("s5xa", [sb("s5_xa%d" % i, [128, D]) for i in range(2)])
    xbr = Ring("s5xb", [sb("s5_xb%d" % i, [128, D]) for i in range(2)])
    xownr = Ring("s5xown", [sb("s5_xown%d" % i, [128, 4, D]) for i in range(2)])
    hring = Ring("s5h", [sb("s5_h%d" % i, [128, 8, 512], BF16) for i in range(2)])
    Wn = prenorm_work(nc, st, "s5n_")
    Wp = postnorm_work(nc, st, "s5p_")
    x2r = Ring("s5x2", [sb("s5_x2s%d" % i, [128, 512]) for i in range(2)])
    tqr = Ring("s5tq", [sb("s5_tq%d" % i, [128, 512]) for i in range(2)])
    sgr = Ring("s5sg", [sb("s5_sg%d" % i, [128, 512]) for i in range(2)])
    hsa = Ring("s5hsa", [sb("s5_hsa%d" % i, [128, 512]) for i in range(2)])
    hsb = Ring("s5hsb", [sb("s5_hsb%d" % i, [128, 512]) for i in range(2)])
    yT = sb("s5_yT", [128, 10, 512], BF16)
    xoring = Ring("s5xo", [sb("s5_xo%d" % i, [128, D]) for i in range(2)])
    pgr = Ring("s5pg", [ps("s5_pg%d" % i, [128, 512]) for i in range(2)])
    pyr = Ring("s5py", [ps("s5_py%d" % i, [128, 512]) for i in range(4)])

    w_in = T["lru_w_in"]
    for i in range(2):
        S.dma("pool", lambda i=i: PO.dma_start(out=wg[:, :, i * 640:(i + 1) * 640],
                                               in_=w_in[:, i * 640:(i + 1) * 640].rearrange("(kc p) n -> p kc n", p=128)), w=["wg"])
        S.dma("pool", lambda i=i: PO.dma_start(out=wo[:, :, i * 512:(i + 1) * 512],
                                               in_=T["lru_w_out"][:, i * 512:(i + 1) * 512].rearrange("(c p) n -> p c n", p=128)), w=["wo"])
    S.dma("sp", lambda: nc.sync.dma_start(out=hsel[:], in_=T["hsel"]), w=["hsel"])
    x1 = R["x1"]
    ctx = {}

    def pn(g):
        hT, kh = hring.next()
        xown, kxw = xownr.next()
        hkeys = []
        for t in range(4):
            ti = 4 * g + t
            xa, kxa = xar.next()
            xb, kxb = xbr.next()
            S.dma("sp", lambda xa=xa, ti=ti: nc.sync.dma_start(out=xa[:], in_=x1[ti * 128:(ti + 1) * 128, :]), w=[kxa])
            S.dma("sp", lambda xb=xb, ti=ti: nc.sync.dma_start(out=xb[:], in_=x1[2048 + ti * 128:2048 + (ti + 1) * 128, :]), w=[kxb])
            kxo = "%s_%d" % (kxw, t)
            S.op("dve", lambda xa=xa, t=t: V.tensor_scalar(out=xown[:, t, :], in0=xa[:], scalar1=hsel[:, 1:2], scalar2=None, op0=ALU.mult),
                 r=[kxa, "hsel"], w=[kxo])
            S.op("dve", lambda xb=xb, t=t: V.scalar_tensor_tensor(out=xown[:, t, :], in0=xb[:], scalar=hsel[:, 0:1], in1=xown[:, t, :],
                                                                  op0=ALU.mult, op1=ALU.add), r=[kxb, "hsel", kxo], w=[kxo])
            kd = "%st%d" % (kh, t)
            prenorm_tile(S, nc, Wn, xown[:, t, :], kxo, 2, P, C, lambda kc, t=t: hT[:, kc, t * 128:(t + 1) * 128], kd)
            hkeys += k8(kd)
        ctx[g] = (hT, hkeys, xown, kxw)

    def chunk(g, c):
        hT, hkeys, xown, kxw = ctx[g]
        pg_, kpg = pgr.next()
        for kc in range(8):
            S.op("pe", lambda kc=kc: PE_.matmul(pg_[:], lhsT=wg[:, kc, c * 128:(c + 1) * 128], rhs=hT[:, kc, :],
                                                start=(kc == 0), stop=(kc == 7)), r=["wg"] + hkeys, w=[kpg])
        ha, kha = hsa.next()
        hb, khb = hsb.next()
        x2s, kx2 = x2r.next()
        tq, ktq = tqr.next()
        sg, ksg = sgr.next()
        S.dma("sp", lambda: nc.sync.dma_start(out=ha[:], in_=R["hs"][c, :, g * 512:(g + 1) * 512]), w=[kha])
        S.dma("sp", lambda: nc.sync.dma_start(out=hb[:], in_=R["hs"][c, :, 2048 + g * 512:2048 + (g + 1) * 512]), w=[khb])
        S.op("act", lambda: A.activation(out=x2s[:], in_=pg_[:], func=AF.Square), r=[kpg], w=[kx2])
        S.op("dve", lambda: V.tensor_scalar(out=tq[:], in0=x2s[:], scalar1=0.044715, scalar2=1.0, op0=ALU.mult, op1=ALU.add),
             r=[kx2], w=[ktq])
        S.op("dve", lambda: V.tensor_tensor(out=tq[:], in0=pg_[:], in1=tq[:], op=ALU.mult), r=[kpg, ktq], w=[ktq])
        S.op("act", lambda: A.activation(out=sg[:], in_=tq[:], func=AF.Sigmoid, scale=1.5957691216057308), r=[ktq], w=[ksg])
        S.op("dve", lambda: V.tensor_tensor(out=sg[:], in0=pg_[:], in1=sg[:], op=ALU.mult), r=[kpg, ksg], w=[ksg])
        S.op("act", lambda: A.activation(out=ha[:], in_=ha[:], func=AF.Copy, scale=hsel[:, 1:2]), r=[kha, "hsel"], w=[kha])
        S.op("dve", lambda: V.scalar_tensor_tensor(out=ha[:], in0=hb[:], scalar=hsel[:, 0:1], in1=ha[:],
                                                   op0=ALU.mult, op1=ALU.add), r=[khb, "hsel", kha], w=[kha])
        S.op("dve", lambda: V.tensor_tensor(out=yT[:, c, :], in0=sg[:], in1=ha[:], op=ALU.mult), r=[ksg, kha], w=["yT%d" % c])

    ykeys = ["yT%d" % c for c in range(10)]

    def outtile(g, t):
        hT, hkeys, xown, kxw = ctx[g]
        ti = 4 * g + t
        pys = []
        for hf in range(2):
            py, kpy = pyr.next()
            pys.append((py, kpy))
            for c in range(10):
                S.op("pe", lambda py=py, c=c, hf=hf: PE_.matmul(py[:], lhsT=yT[:, c, t * 128:(t + 1) * 128],
                                                              rhs=wo[:, c, hf * 512:(hf + 1) * 512], start=(c == 0), stop=(c == 9)),
                     r=ykeys + ["wo"], w=[kpy])
        xo, kxo2 = xoring.next()
        postnorm_tile(S, nc, Wp, lambda hf: pys[hf][0][:], [pys[0][1], pys[1][1]], xown[:, t, :], "%s_%d" % (kxw, t), xo, kxo2, 2, P)
        S.dma("pool", lambda: PO.dma_start(out=R["x2"][ti * 128:(ti + 1) * 128, :], in_=xo[:]), r=[kxo2])

    S.replay([S.capture(lambda: pn(0))])
    for g in range(4):
        tn = S.capture(lambda: pn(g + 1)) if g + 1 < 4 else []
        nsl = 7
        sl = [tn[i * len(tn) // nsl:(i + 1) * len(tn) // nsl] for i in range(nsl)]
        for p in range(5):
            S.replay([S.capture(lambda: chunk(g, 2 * p)), S.capture(lambda: chunk(g, 2 * p + 1)), sl[p]])
        for p in range(2):
            S.replay([S.capture(lambda: outtile(g, 2 * p)), S.capture(lambda: outtile(g, 2 * p + 1)), sl[5 + p]])
    S.flush()
    st.close()


def host_consts(half):
    bf = ml_dtypes.bfloat16
    c = {}
    c["ident"] = np.eye(128, dtype=np.float32)
    c["identb"] = np.eye(128, dtype=np.float32).astype(bf)
    m = np.zeros((128, 128), np.float32)
    for hd in range(2):
        for i in range(8):
            m[hd * 64 + i + 8, hd * 64 + i] = -1.0
            m[hd * 64 + i, hd * 64 + i + 8] = 1.0
    c["msw"] = m.astype(bf)
    pos = np.arange(SEQ, dtype=np.float32)
    inv = np.power(np.float32(500000.0), -np.arange(8, dtype=np.float32) / np.float32(8.0)).astype(np.float32)
    ang = pos[None, :] * inv[:, None]
    ct = np.ones((128, SEQ), np.float32)
    stt = np.zeros((128, SEQ), np.float32)
    for hd in range(2):
        for r in range(16):
            ct[hd * 64 + r] = np.cos(ang[r % 8])
            stt[hd * 64 + r] = np.sin(ang[r % 8])
    c["ropeC"], c["ropeS"] = ct, stt
    c["utri"] = np.triu(np.ones((128, 128), np.float32))
    mk_ = np.zeros((128, 4, 512), np.float32)
    kk = np.arange(128)[:, None]
    qq = np.arange(512)[None, :]
    for i in range(4):
        mk_[:, i, :] = np.where(i * 128 + kk <= qq, 0.0, -30000.0)
    c["maskc"] = mk_.reshape(128, 2048).astype(bf)
    bo = np.zeros((16, SEQ), np.float32)
    for n in range(16):
        bo[n, n * 256:(n + 1) * 256] = 1.0
    c["BO"] = bo.astype(bf)
    fo = np.zeros((16, SEQ), np.float32)
    fo[0] = 1.0
    c["FO"] = fo.astype(bf)
    gm = np.zeros((128, 16, 8, 16), np.float32)
    for nv in range(16):
        gm[:, nv, :, nv:] = -1e30
    c["gmask"] = gm.reshape(128, 16 * 128)
    c["hsel"] = np.full((128, 2), float(half), np.float32)
    c["hsel"][:, 1] = 1.0 - float(half)
    return c


CONST_SPECS = [("ident", [128, 128], F32), ("identb", [128, 128], BF16), ("msw", [128, 128], BF16), ("ropeC", [128, SEQ], F32),
               ("ropeS", [128, SEQ], F32), ("utri", [128, 128], F32), ("maskc", [128, 2048], BF16), ("BO", [16, SEQ], BF16),
               ("FO", [16, SEQ], BF16), ("hsel", [128, 2], F32), ("gmask", [128, 2048], F32)]


def build(upto=99, dbg=False, want_out=False):
    nc = bass.Bass("TRN2", target_bir_lowering=False)
    T = {}
    inp = lambda name, shape, dt=F32: nc.dram_tensor(name, shape, dt, kind="ExternalInput").ap()
    T["x"] = inp("x", [SEQ, D])
    T["c"] = inp("c", [1, D])
    T["w_ada"] = inp("w_ada", [2, D, 6 * D])
    T["b_ada"] = inp("b_ada", [1, 2 * 6 * D])
    T["norm_g"] = inp("norm_g", [1, 8 * D])
    T["attn_w_in"] = inp("attn_w_in", [D, 3080])
    T["fox_b_f"] = inp("fox_b_f", [1, 8])
    T["attn_w_out"] = inp("attn_w_out", [D, D])
    T["ffn_w_gate"] = inp("ffn_w_gate", [D, 2816])
    T["ffn_w_up"] = inp("ffn_w_up", [D, 2816])
    T["ffn_w_down"] = inp("ffn_w_down", [2816, D])
    T["lru_w_in"] = inp("lru_w_in", [D, 2560])
    T["lru_conv_w"] = inp("lru_conv_w", [1, 4 * 1280])
    for nm in ("lru_conv_b", "lru_b_a", "lru_b_x", "lru_lambda"):
        T[nm] = inp(nm, [1, 1280])
    T["lru_w_a"] = inp("lru_w_a", [10, 128, 128])
    T["lru_w_x"] = inp("lru_w_x", [10, 128, 128])
    T["lru_w_out"] = inp("lru_w_out", [1280, D])
    T["moe_w_router"] = inp("moe_w_router", [D, 8])
    T["moe_b_router"] = inp("moe_b_router", [1, 8])
    T["moe_w_gate"] = inp("moe_w_gate", [8, D, 3584])
    T["moe_w_up"] = inp("moe_w_up", [8, D, 3584])
    T["moe_w_down"] = inp("moe_w_down", [8, 3584, D])
    for name, shape, dt in CONST_SPECS:
        T[name] = inp(name, shape, dt)
    kind = "ExternalOutput" if dbg else "Internal"
    scr = lambda name, shape, dt=F32: nc.dram_tensor(name, shape, dt, kind=kind).ap()
    R = {}
    R["QT"] = scr("r_QT", [8, 128, SEQ], BF16)
    R["KT"] = scr("r_KT", [8, 128, SEQ], BF16)
    R["V"] = scr("r_V", [NT, 128, 16 * 65], BF16)
    R["NB"] = scr("r_NB", [128, 8 * NT * 8])
    R["RQ"] = scr("r_RQ", [8, SEQ], BF16)
    R["NS"] = scr("r_NS", [16, 8, SEQ], BF16)
    R["O"] = scr("r_O", [SEQ, D], BF16)
    R["x1a"] = scr("r_x1a", [SEQ, D])
    R["x1"] = scr("r_x1", [SEQ, D])
    R["hs"] = scr("r_hs", [10, 128, SEQ])
    R["x2"] = scr("r_x2", [2048, D])
    R["out"] = nc.dram_tensor("out", [2048, D], F32, kind="ExternalOutput").ap()
    bar = nc.dram_tensor("bar_scratch", [2, 16], F32, kind="Internal").ap()
    es = ExitStack()
    S = Sched(nc, es)
    S.bar_src = bar[0:1, :]
    S.bar_dst = bar[1:2, :]
    P = {}
    P["AT"] = es.enter_context(nc.sbuf_tensor("P_AT", [128, 4, 8], F32))
    P["shT"] = es.enter_context(nc.sbuf_tensor("P_shT", [128, 4, 8], F32))
    P["Brep"] = es.enter_context(nc.sbuf_tensor("P_Brep", [128, 4, D], F32))
    C = {}
    C["ident"] = es.enter_context(nc.sbuf_tensor("C_ident", [128, 128], F32))
    C["identb"] = es.enter_context(nc.sbuf_tensor("C_identb", [128, 128], BF16))
    S.dma("sp", lambda: nc.sync.dma_start(out=C["ident"][:], in_=T["ident"]), w=["ident"])
    S.dma("sp", lambda: nc.sync.dma_start(out=C["identb"][:], in_=T["identb"]), w=["identb"])
    S.dma("sp", lambda: nc.sync.dma_start(out=bar[:, :], in_=C["ident"][0:2, 0:16]), r=["ident"])
    stage0(S, nc, es, T, P)
    if upto >= 1:
        stage1(S, nc, T, P, C, R)
    if upto >= 2:
        stage2(S, nc, T, P, C, R)
    if upto >= 3:
        stage3a(S, nc, T, P, C, R)
        ffn = [(T["ffn_w_gate"], T["ffn_w_up"], T["ffn_w_down"])]
        ffn_pass(S, nc, T, P, C, "f0a_", R["x1a"][0:2048, :], R["x1"][0:2048, :], 1, ffn, 2816)
        ffn_pass(S, nc, T, P, C, "f0b_", R["x1a"][2048:4096, :], R["x1"][2048:4096, :], 1, ffn, 2816)
    if upto >= 4:
        stage4(S, nc, T, P, C, R)
    if upto >= 5:
        stage5(S, nc, T, P, C, R)
    if upto >= 6:
        experts = [(T["moe_w_gate"][e], T["moe_w_up"][e], T["moe_w_down"][e]) for e in range(NEXP)]
        ffn_pass(S, nc, T, P, C, "moe_", R["x2"], R["out"], 3, experts, 3584, router=(T["moe_w_router"], T["moe_b_router"]))
    es.close()
    print("instructions emitted:", S.nemit, flush=True)
    return nc


def make_in_maps(inputs):
    maps = []
    f = lambda a: np.ascontiguousarray(a, dtype=np.float32)
    for core in range(8):
        b, half = core // 2, core % 2
        m = {"x": f(inputs["x"][b]), "c": f(inputs["c"][b:b + 1]), "w_ada": f(inputs["w_ada"]),
             "b_ada": f(inputs["b_ada"]).reshape(1, -1), "norm_g": f(inputs["norm_g"]).reshape(1, -1),
             "attn_w_in": f(inputs["attn_w_in"][0]), "fox_b_f": f(inputs["fox_b_f"]).reshape(1, 8),
             "attn_w_out": f(inputs["attn_w_out"][0]), "ffn_w_gate": f(inputs["ffn_w_gate"][0]),
             "ffn_w_up": f(inputs["ffn_w_up"][0]), "ffn_w_down": f(inputs["ffn_w_down"][0]),
             "lru_w_in": f(inputs["lru_w_in"][0]), "lru_conv_w": f(inputs["lru_conv_w"][0]).reshape(1, -1),
             "lru_conv_b": f(inputs["lru_conv_b"]).reshape(1, -1), "lru_b_a": f(inputs["lru_b_a"]).reshape(1, -1),
             "lru_b_x": f(inputs["lru_b_x"]).reshape(1, -1), "lru_lambda": f(inputs["lru_lambda"]).reshape(1, -1),
             "lru_w_a": f(inputs["lru_w_a"][0]), "lru_w_x": f(inputs["lru_w_x"][0]), "lru_w_out": f(inputs["lru_w_out"][0]),
             "moe_w_router": f(inputs["moe_w_router"][0]), "moe_b_router": f(inputs["moe_b_router"]).reshape(1, 8),
             "moe_w_gate": f(inputs["moe_w_gate"][0]), "moe_w_up": f(inputs["moe_w_up"][0]), "moe_w_down": f(inputs["moe_w_down"][0])}
        m.update(host_consts(half))
        maps.append(m)
    return maps


def kernel(**inputs):
    nc = build(upto=6, dbg=False)
    maps = make_in_maps(inputs)
    res = run_bass_kernel_spmd(nc, maps, core_ids=list(range(8)))
    out = np.zeros((4, SEQ, D), np.float32)
    for core in range(8):
        b, half = core // 2, core % 2
        out[b, half * 2048:(half + 1) * 2048] = np.asarray(res.results[core]["out"], dtype=np.float32)
    return out
```
